# Optimizing a Trainium2 kernel written in Bass

```python
import jax, jax.numpy as jnp
from jax import lax
import numpy as np

D_MODEL = 1024
BATCH = 8
SEQ = 4096
DEPTH = 2

HEAD_DIM = 64
C_ATT = D_MODEL // 4
C_RWKV = (D_MODEL - C_ATT) // 2
C_MLSTM = D_MODEL - C_ATT - C_RWKV
D_MIX = C_ATT + C_RWKV + C_MLSTM
H_ATT = C_ATT // HEAD_DIM
H_RWKV = C_RWKV // HEAD_DIM
H_MLSTM = C_MLSTM // HEAD_DIM
CMP_BLOCK = 32
CMP_STRIDE = 16
SLC_BLOCK = 64
N_SELECT = 16
WINDOW = 512
Q_BLOCK = 128
NEG = -1e30
FORCE = 1e9
RANK_W = 64
RANK_A = 64
RANK_G = 128
RWKV_GN_EPS = 64e-5
MLSTM_CHUNK = 64
CONV_WIDTH = 4
D_FF = -(-8 * D_MODEL // (3 * 256)) * 256
NORM_EPS = 1e-6
NSA_SIZES = (C_ATT, HEAD_DIM, HEAD_DIM, HEAD_DIM, HEAD_DIM, HEAD_DIM, HEAD_DIM, 3 * H_ATT)
RWKV_SIZES = (C_RWKV, C_RWKV, C_RWKV, RANK_W, RANK_A, RANK_G)
MLSTM_SIZES = (2 * C_MLSTM, C_MLSTM, C_MLSTM, H_MLSTM, H_MLSTM)
NSA_IN = sum(NSA_SIZES)
RWKV_IN = sum(RWKV_SIZES)
MLSTM_IN = sum(MLSTM_SIZES)
D_IN = NSA_IN + RWKV_IN + MLSTM_IN

kernel_name = "hybrid_nsa_rwkv7_mlstm_block"


def split_cols(t, sizes):
    points = np.cumsum(np.array(sizes))[:-1].tolist()
    return jnp.split(t, points, axis=-1)


def rms_norm(x, g, eps=NORM_EPS):
    x32 = x.astype(jnp.float32)
    y = x32 * lax.rsqrt(jnp.mean(x32 * x32, axis=-1, keepdims=True) + eps)
    return (y * g.astype(jnp.float32)).astype(x.dtype)


def shift_right(t):
    return jnp.pad(t, ((0, 0), (1, 0), (0, 0)))[:, :-1]


def causal_dwconv(t, w, b):
    k = w.shape[0]
    out = lax.conv_general_dilated(t, w[:, None, :], window_strides=(1,), padding=[(k - 1, 0)],
                                   dimension_numbers=("NWC", "WIO", "NWC"),
                                   feature_group_count=t.shape[-1])
    return out + b


def nsa_mixer(q, kc, vc, ks, vs, kw, vw, gts, gate_b, pe_k, pe_v, ck_w1, ck_w2, cv_w1, cv_w2):
    B, S, _ = q.shape
    f32 = jnp.float32
    q = q.reshape(B, S, H_ATT, HEAD_DIM)
    scale = HEAD_DIM ** -0.5
    pos = jnp.arange(S)
    n_cmp = (S - CMP_BLOCK) // CMP_STRIDE + 1
    cmp_start = jnp.arange(n_cmp) * CMP_STRIDE
    blk_idx = cmp_start[:, None] + jnp.arange(CMP_BLOCK)[None, :]

    def compress(t, pe, w1, w2):
        tb = t[:, blk_idx] + pe
        return jax.nn.gelu(tb.reshape(B, n_cmp, CMP_BLOCK * HEAD_DIM) @ w1) @ w2

    k_cmp = compress(kc, pe_k, ck_w1, ck_w2)
    v_cmp = compress(vc, pe_v, cv_w1, cv_w2)
    cmp_mask = (cmp_start + CMP_BLOCK - 1)[None, :] <= pos[:, None]
    s_cmp = jnp.einsum('bshd,bnd->bhsn', q, k_cmp).astype(f32) * scale
    p_cmp = jax.nn.softmax(jnp.where(cmp_mask, s_cmp, NEG), axis=-1) * cmp_mask
    o_cmp = jnp.einsum('bhsn,bnd->bshd', p_cmp.astype(q.dtype), v_cmp)
    n_slc = S // SLC_BLOCK
    n_sel = min(N_SELECT, n_slc)
    slc_start = jnp.arange(n_slc) * SLC_BLOCK
    overlap = ((cmp_start[:, None] < slc_start[None, :] + SLC_BLOCK)
               & (cmp_start[:, None] + CMP_BLOCK > slc_start[None, :])).astype(f32)
    imp = jnp.einsum('bhsn,nj->bsj', p_cmp, overlap)
    cur = pos // SLC_BLOCK
    j = jnp.arange(n_slc)
    forced = (j[None, :] == 0) | (j[None, :] == cur[:, None]) | (j[None, :] == cur[:, None] - 1)
    valid = j[None, :] <= cur[:, None]
    score = jnp.where(forced, FORCE, jnp.where(valid, imp, NEG))
    _, sel_idx = lax.top_k(score, n_sel)
    ks_blk = ks.reshape(B, n_slc, SLC_BLOCK, HEAD_DIM)
    vs_blk = vs.reshape(B, n_slc, SLC_BLOCK, HEAD_DIM)
    kw_pad = jnp.pad(kw, ((0, 0), (WINDOW, 0), (0, 0)))
    vw_pad = jnp.pad(vw, ((0, 0), (WINDOW, 0), (0, 0)))
    span = WINDOW + Q_BLOCK
    b_ix = jnp.arange(B)[:, None, None]

    def block(i):
        q0 = i * Q_BLOCK
        qb = lax.dynamic_slice_in_dim(q, q0, Q_BLOCK, axis=1)
        idx = lax.dynamic_slice_in_dim(sel_idx, q0, Q_BLOCK, axis=1)
        qpos = q0 + jnp.arange(Q_BLOCK)
        kg = ks_blk[b_ix, idx]
        vg = vs_blk[b_ix, idx]
        kpos = idx[..., None] * SLC_BLOCK + jnp.arange(SLC_BLOCK)
        m_s = kpos <= qpos[None, :, None, None]
        s_s = jnp.einsum('bqhd,bqnkd->bhqnk', qb, kg).astype(f32) * scale
        s_s = jnp.where(m_s[:, None], s_s, NEG).reshape(B, H_ATT, Q_BLOCK, n_sel * SLC_BLOCK)
        p_s = jax.nn.softmax(s_s, axis=-1).reshape(B, H_ATT, Q_BLOCK, n_sel, SLC_BLOCK)
        o_s = jnp.einsum('bhqnk,bqnkd->bqhd', p_s.astype(qb.dtype), vg)
        kwb = lax.dynamic_slice_in_dim(kw_pad, q0, span, axis=1)
        vwb = lax.dynamic_slice_in_dim(vw_pad, q0, span, axis=1)
        wpos = q0 - WINDOW + jnp.arange(span)
        m_w = ((wpos[None, :] <= qpos[:, None]) & (wpos[None, :] > qpos[:, None] - WINDOW)
               & (wpos[None, :] >= 0))
        s_w = jnp.einsum('bqhd,bkd->bhqk', qb, kwb).astype(f32) * scale
        p_w = jax.nn.softmax(jnp.where(m_w, s_w, NEG), axis=-1)
        o_w = jnp.einsum('bhqk,bkd->bqhd', p_w.astype(qb.dtype), vwb)
        return o_s, o_w

    o_s, o_w = lax.map(block, jnp.arange(S // Q_BLOCK))
    o_s = o_s.transpose(1, 0, 2, 3, 4).reshape(B, S, H_ATT, HEAD_DIM)
    o_w = o_w.transpose(1, 0, 2, 3, 4).reshape(B, S, H_ATT, HEAD_DIM)
    g = jax.nn.sigmoid(gts + gate_b).reshape(B, S, 3, H_ATT)[..., None]
    o = g[:, :, 0] * o_cmp + g[:, :, 1] * o_s + g[:, :, 2] * o_w
    return o.reshape(B, S, C_ATT)


def rwkv7_mixer(r, k, v, xw, xa, xg, w0, w2, a0, a2, g2, k_k, k_a, r_k, ln_w, ln_b):
    B, S, _ = r.shape
    H, N = H_RWKV, HEAD_DIM
    f32 = jnp.float32
    w = -jax.nn.softplus(-(w0 + jnp.tanh(xw) @ w2)) - 0.5
    decay = jnp.exp(-jnp.exp(w.astype(f32)))
    a = jax.nn.sigmoid(a0 + xa @ a2)
    g = jax.nn.sigmoid(xg) @ g2
    heads = lambda t: t.reshape(B, S, H, N).astype(f32)
    kk = heads(k * k_k)
    kk = kk / jnp.maximum(jnp.sqrt(jnp.sum(kk * kk, axis=-1, keepdims=True)), 1e-12)
    k = k * (1 + (a - 1) * k_a)
    rh, kh, vh, ah, wh = heads(r), heads(k), heads(v), heads(a), heads(decay)

    def step(state, inp):
        r_t, w_t, k_t, v_t, kk_t, a_t = inp
        sa = jnp.einsum('bhij,bhj->bhi', state, -kk_t)
        state = (state * w_t[:, :, None, :] + sa[..., None] * (kk_t * a_t)[:, :, None, :]
                 + v_t[..., None] * k_t[:, :, None, :])
        return state, jnp.einsum('bhij,bhj->bhi', state, r_t)

    xs = tuple(t.transpose(1, 0, 2, 3) for t in (rh, wh, kh, vh, kk, ah))
    _, y = lax.scan(step, jnp.zeros((B, H, N, N), f32), xs)
    y = y.transpose(1, 0, 2, 3)
    mu = jnp.mean(y, axis=-1, keepdims=True)
    var = jnp.mean((y - mu) ** 2, axis=-1, keepdims=True)
    y = ((y - mu) * lax.rsqrt(var + RWKV_GN_EPS)).reshape(B, S, C_RWKV) * ln_w + ln_b
    bonus = jnp.sum(rh * kh * r_k.astype(f32), axis=-1, keepdims=True) * vh
    y = y + bonus.reshape(B, S, C_RWKV)
    return (y * g).astype(r.dtype)


def mlstm_mixer(q, k, v, o, ig, fg, ig_b, fg_b, norm_g):
    B, S, _ = q.shape
    H, Dh, L = H_MLSTM, HEAD_DIM, MLSTM_CHUNK
    nc = S // L
    f32 = jnp.float32
    chunks = lambda t: t.reshape(B, nc, L, H, Dh).transpose(0, 3, 1, 2, 4).astype(f32)
    gate = lambda t: t.astype(f32).reshape(B, nc, L, H).transpose(0, 3, 1, 2)
    qc, kc, vc = chunks(q), chunks(k) * (Dh ** -0.5), chunks(v)
    log_i = gate(ig + ig_b)
    log_f = jax.nn.log_sigmoid(gate(fg + fg_b))
    b = jnp.cumsum(log_f, axis=-1)
    g_tot = b[..., -1]
    u = g_tot[..., None] - b + log_i

    def step(carry, inp):
        C, n, m = carry
        g_c, u_c, k_c, v_c = inp
        m_new = jnp.maximum(g_c + m, jnp.max(u_c, axis=-1))
        dec = jnp.exp(g_c + m - m_new)
        wgt = jnp.exp(u_c - m_new[..., None])
        C_new = dec[..., None, None] * C + jnp.einsum('bhs,bhsd,bhse->bhde', wgt, v_c, k_c)
        n_new = dec[..., None] * n + jnp.einsum('bhs,bhse->bhe', wgt, k_c)
        return (C_new, n_new, m_new), (C, n, m)

    front = lambda t: jnp.moveaxis(t, 2, 0)
    init = (jnp.zeros((B, H, Dh, Dh), f32), jnp.zeros((B, H, Dh), f32), jnp.zeros((B, H), f32))
    _, (C0, n0, m0) = lax.scan(step, init, (front(g_tot), front(u), front(kc), front(vc)))
    C0, n0, m0 = jnp.moveaxis(C0, 0, 2), jnp.moveaxis(n0, 0, 2), jnp.moveaxis(m0, 0, 2)
    a_inter = b + m0[..., None]
    causal = jnp.tril(jnp.ones((L, L), dtype=bool))
    D = jnp.where(causal, b[..., :, None] - b[..., None, :] + log_i[..., None, :], -jnp.inf)
    m_t = jnp.maximum(a_inter, jnp.max(D, axis=-1))
    w_inter = jnp.exp(a_inter - m_t)
    s = jnp.exp(D - m_t[..., None]) * jnp.einsum('bhcjd,bhcsd->bhcjs', qc, kc)
    num = (w_inter[..., None] * jnp.einsum('bhcde,bhcje->bhcjd', C0, qc)
           + jnp.einsum('bhcjs,bhcsd->bhcjd', s, vc))
    den = w_inter * jnp.einsum('bhce,bhcje->bhcj', n0, qc) + jnp.sum(s, axis=-1)
    h = num / jnp.maximum(jnp.abs(den), jnp.exp(-m_t))[..., None]
    h = h * lax.rsqrt(jnp.mean(h * h, axis=-1, keepdims=True) + NORM_EPS)
    h = h.transpose(0, 2, 3, 1, 4).reshape(B, S, C_MLSTM) * norm_g.astype(f32)
    return (jax.nn.sigmoid(o.astype(f32)) * h).astype(q.dtype)


def hybrid_mixer(h, w_in, w_out, nsa_pe_k, nsa_pe_v, nsa_ck_w1, nsa_ck_w2, nsa_cv_w1, nsa_cv_w2,
                 nsa_gate_b, nsa_out_g, rw_mu, rw_w0, rw_w2, rw_a0, rw_a2, rw_g2, rw_kk, rw_ka,
                 rw_rk, rw_ln_w, rw_ln_b, ml_conv_w, ml_conv_b, ml_ig_b, ml_fg_b, ml_norm_g):
    proj = h @ w_in
    p_nsa, p_rw, p_ml = split_cols(proj, (NSA_IN, RWKV_IN, MLSTM_IN))
    q, kc, vc, ks, vs, kw, vw, gts = split_cols(p_nsa, NSA_SIZES)
    o_nsa = nsa_mixer(q, kc, vc, ks, vs, kw, vw, gts, nsa_gate_b, nsa_pe_k, nsa_pe_v,
                      nsa_ck_w1, nsa_ck_w2, nsa_cv_w1, nsa_cv_w2)
    o_nsa = rms_norm(o_nsa, nsa_out_g)
    p_rw = p_rw + rw_mu * (shift_right(p_rw) - p_rw)
    r, k, v, xw, xa, xg = split_cols(p_rw, RWKV_SIZES)
    o_rw = rwkv7_mixer(r, k, v, xw, xa, xg, rw_w0, rw_w2, rw_a0, rw_a2, rw_g2, rw_kk, rw_ka,
                       rw_rk, rw_ln_w, rw_ln_b)
    qk, mv, mo, ig, fg = split_cols(p_ml, MLSTM_SIZES)
    qk = jax.nn.silu(causal_dwconv(qk, ml_conv_w, ml_conv_b))
    mq, mk = jnp.split(qk, 2, axis=-1)
    o_ml = mlstm_mixer(mq, mk, mv, mo, ig, fg, ml_ig_b, ml_fg_b, ml_norm_g)
    return jnp.concatenate([o_nsa, o_rw, o_ml], axis=-1) @ w_out


def swiglu(h, w_gate, w_up, w_down):
    return (jax.nn.silu(h @ w_gate) * (h @ w_up)) @ w_down


def setup_inputs(seed: int = 0) -> dict:
    key = jax.random.key(seed)
    keys = jax.random.split(key, 40)
    counter = [0]

    def nxt():
        kk = keys[counter[0]]
        counter[0] += 1
        return kk

    f32 = jnp.float32
    L = DEPTH
    nrm = lambda shape, s: s * jax.random.normal(nxt(), shape, f32)
    gain = lambda shape: 1.0 + 0.05 * jax.random.normal(nxt(), shape, f32)
    unif = lambda shape, lo, hi: jax.random.uniform(nxt(), shape, f32, lo, hi)
    return {
        "x": nrm((BATCH, SEQ, D_MODEL), 1.0),
        "c": nrm((BATCH, D_MODEL), 1.0),
        "w_mod": nrm((L, D_MODEL, 6 * D_MODEL), 0.3 * D_MODEL ** -0.5),
        "b_mod": nrm((L, 6 * D_MODEL), 0.02),
        "g_pre_mix": gain((L, D_MODEL)),
        "g_post_mix": gain((L, D_MODEL)),
        "g_pre_ffn": gain((L, D_MODEL)),
        "g_post_ffn": gain((L, D_MODEL)),
        "w_in": nrm((L, D_MODEL, D_IN), D_MODEL ** -0.5),
        "w_out": nrm((L, D_MIX, D_MODEL), D_MIX ** -0.5),
        "nsa_pe_k": nrm((L, CMP_BLOCK, HEAD_DIM), 0.1),
        "nsa_pe_v": nrm((L, CMP_BLOCK, HEAD_DIM), 0.1),
        "nsa_ck_w1": nrm((L, CMP_BLOCK * HEAD_DIM, HEAD_DIM), (CMP_BLOCK * HEAD_DIM) ** -0.5),
        "nsa_ck_w2": nrm((L, HEAD_DIM, HEAD_DIM), HEAD_DIM ** -0.5),
        "nsa_cv_w1": nrm((L, CMP_BLOCK * HEAD_DIM, HEAD_DIM), (CMP_BLOCK * HEAD_DIM) ** -0.5),
        "nsa_cv_w2": nrm((L, HEAD_DIM, HEAD_DIM), HEAD_DIM ** -0.5),
        "nsa_gate_b": nrm((L, 3 * H_ATT), 0.1),
        "nsa_out_g": gain((L, C_ATT)),
        "rw_mu": unif((L, RWKV_IN), 0.0, 1.0),
        "rw_w0": unif((L, C_RWKV), -6.5, -1.0),
        "rw_w2": nrm((L, RANK_W, C_RWKV), 0.1 * RANK_W ** -0.5),
        "rw_a0": nrm((L, C_RWKV), 0.1),
        "rw_a2": nrm((L, RANK_A, C_RWKV), 0.1 * RANK_A ** -0.5),
        "rw_g2": nrm((L, RANK_G, C_RWKV), RANK_G ** -0.5),
        "rw_kk": 0.85 + 0.05 * jax.random.normal(nxt(), (L, C_RWKV), f32),
        "rw_ka": gain((L, C_RWKV)),
        "rw_rk": nrm((L, H_RWKV, HEAD_DIM), 0.1),
        "rw_ln_w": gain((L, C_RWKV)),
        "rw_ln_b": nrm((L, C_RWKV), 0.02),
        "ml_conv_w": nrm((L, CONV_WIDTH, 2 * C_MLSTM), CONV_WIDTH ** -0.5),
        "ml_conv_b": nrm((L, 2 * C_MLSTM), 0.02),
        "ml_ig_b": nrm((L, H_MLSTM), 0.1),
        "ml_fg_b": unif((L, H_MLSTM), 3.0, 6.0),
        "ml_norm_g": gain((L, C_MLSTM)),
        "ffn_w_gate": nrm((L, D_MODEL, D_FF), D_MODEL ** -0.5),
        "ffn_w_up": nrm((L, D_MODEL, D_FF), D_MODEL ** -0.5),
        "ffn_w_down": nrm((L, D_FF, D_MODEL), D_FF ** -0.5),
    }


def reference(x, c, w_mod, b_mod, g_pre_mix, g_post_mix, g_pre_ffn, g_post_ffn, w_in, w_out,
              nsa_pe_k, nsa_pe_v, nsa_ck_w1, nsa_ck_w2, nsa_cv_w1, nsa_cv_w2, nsa_gate_b, nsa_out_g,
              rw_mu, rw_w0, rw_w2, rw_a0, rw_a2, rw_g2, rw_kk, rw_ka, rw_rk, rw_ln_w, rw_ln_b,
              ml_conv_w, ml_conv_b, ml_ig_b, ml_fg_b, ml_norm_g, ffn_w_gate, ffn_w_up, ffn_w_down):
    cs = jax.nn.silu(c)
    for l in range(DEPTH):
        mod = cs @ w_mod[l] + b_mod[l]
        sh1, sc1, gt1, sh2, sc2, gt2 = [m[:, None, :] for m in jnp.split(mod, 6, axis=-1)]
        h = rms_norm(x, g_pre_mix[l]) * (1 + sc1) + sh1
        y = hybrid_mixer(h, w_in[l], w_out[l], nsa_pe_k[l], nsa_pe_v[l], nsa_ck_w1[l], nsa_ck_w2[l],
                         nsa_cv_w1[l], nsa_cv_w2[l], nsa_gate_b[l], nsa_out_g[l], rw_mu[l], rw_w0[l],
                         rw_w2[l], rw_a0[l], rw_a2[l], rw_g2[l], rw_kk[l], rw_ka[l], rw_rk[l],
                         rw_ln_w[l], rw_ln_b[l], ml_conv_w[l], ml_conv_b[l], ml_ig_b[l], ml_fg_b[l],
                         ml_norm_g[l])
        x = x + gt1 * rms_norm(y, g_post_mix[l])
        h = rms_norm(x, g_pre_ffn[l]) * (1 + sc2) + sh2
        y = swiglu(h, ffn_w_gate[l], ffn_w_up[l], ffn_w_down[l])
        x = x + gt2 * rms_norm(y, g_post_ffn[l])
    return x
```

```python
import numpy as np
from contextlib import ExitStack
import concourse.bass as bass
import concourse.mybir as mybir
from concourse.bass_utils import run_bass_kernel_spmd

F32 = mybir.dt.float32
BF16 = mybir.dt.bfloat16
AF = mybir.ActivationFunctionType
ALU = mybir.AluOpType
AX = mybir.AxisListType

D = 1024
DFF = 2816
DIN = 3608
NEGB = -30000.0
N_DMA_SEMS = 16
N_SDMA_SEMS = 8
SAME_ENGINE_SYNC = True


def _box(ap):
    t = ap.tensor
    pat = ap.ap
    off = int(ap.offset)
    fsz = 1
    for s in t.shape[1:]:
        fsz *= int(s)
    pstep, pcnt = pat[0]
    p0 = off // fsz
    f0 = off % fsz
    p1 = p0 + 1 if pstep == 0 else p0 + (pcnt - 1) * (pstep // fsz) + 1
    ext = 0
    for st, cnt in pat[1:]:
        ext += abs(st) * (cnt - 1)
    if t.name.startswith('ps'):
        return (t.name, 0, 128, 0, 1 << 20)
    return (t.name, p0, p1, f0, f0 + ext + 1)


class Prog:
    def __init__(self, nc, st):
        self.nc = nc
        self.engs = {'pe': nc.tensor, 'act': nc.scalar, 'dve': nc.vector, 'pool': nc.gpsimd, 'sp': nc.sync}
        self.q = {e: [] for e in self.engs}
        self.tick = {e: 0 for e in self.engs}
        self.recs = {}
        self.seen = {e: {} for e in self.engs}
        self.dma_tot = {}
        self.dma_next = {'h': 0, 's': 0}
        self.sem = {}
        for e in self.engs:
            self.sem[e] = st.enter_context(nc.semaphore('s_' + e))
        for k in range(N_DMA_SEMS):
            self.sem['dma%d' % k] = st.enter_context(nc.semaphore('s_dma%d' % k))
            self.dma_tot['dma%d' % k] = 0
        for k in range(N_SDMA_SEMS):
            self.sem['sdma%d' % k] = st.enter_context(nc.semaphore('s_sdma%d' % k))
            self.dma_tot['sdma%d' % k] = 0

    def _deps(self, reads, writes):
        waits = {}
        for (is_w, lst) in ((False, reads), (True, writes)):
            for (key, p0, p1, f0, f1) in lst:
                for r in self.recs.get(key, ()):
                    if r[0] < p1 and p0 < r[1] and r[2] < f1 and f0 < r[3] and (is_w or r[6]):
                        if waits.get(r[4], 0) < r[5]:
                            waits[r[4]] = r[5]
        return waits

    def _record(self, semname, val, reads, writes):
        for (is_w, lst) in ((False, reads), (True, writes)):
            for (key, p0, p1, f0, f1) in lst:
                L = self.recs.get(key, [])
                newL = []
                for r in L:
                    cov = (p0 <= r[0] and r[1] <= p1 and f0 <= r[2] and r[3] <= f1)
                    if cov and (is_w or (not r[6] and r[4] == semname)):
                        continue
                    newL.append(r)
                newL.append([p0, p1, f0, f1, semname, val, is_w])
                self.recs[key] = newL

    @staticmethod
    def _boxes(lst):
        out = []
        for x in lst:
            if x is None or isinstance(x, (int, float)):
                continue
            out.append(x if isinstance(x, tuple) else _box(x))
        return out

    def op(self, eng, fn, reads=(), writes=()):
        reads = self._boxes(reads)
        writes = self._boxes(writes)
        waits = self._deps(reads, writes)
        if not SAME_ENGINE_SYNC or eng == 'pe':
            waits.pop(eng, None)
        self.tick[eng] += 1
        self.q[eng].append((waits, fn, (eng, 1)))
        self._record(eng, self.tick[eng], reads, writes)

    def dma(self, eng, out, in_, reads=None, writes=None, **kw):
        r = self._boxes(reads if reads is not None else [in_])
        w = self._boxes(writes if writes is not None else [out])
        waits = self._deps(r, w)
        if eng == 'pool':
            sname = 'sdma%d' % (self.dma_next['s'] % N_SDMA_SEMS)
            self.dma_next['s'] += 1
        else:
            sname = 'dma%d' % (self.dma_next['h'] % N_DMA_SEMS)
            self.dma_next['h'] += 1
        if self.dma_tot[sname] > 0 and waits.get(sname, 0) < self.dma_tot[sname]:
            waits[sname] = self.dma_tot[sname]
        self.dma_tot[sname] += 16
        self.q[eng].append((waits, (lambda e, o=out, i=in_, kw=kw: e.dma_start(out=o, in_=i, **kw)), (sname, 16)))
        self._record(sname, self.dma_tot[sname], r, w)

    def emit(self):
        nc = self.nc
        final = {e: self.tick[e] for e in self.engs}
        dfinal = dict(self.dma_tot)
        with nc.Block() as block:
            def run(ename, engine):
                seen = self.seen[ename]
                for waits, fn, inc in self.q[ename]:
                    for s, v in waits.items():
                        if seen.get(s, 0) >= v:
                            continue
                        seen[s] = v
                        engine.wait_ge(self.sem[s], v)
                    fn(engine).then_inc(self.sem[inc[0]], inc[1])
                for s, v in final.items():
                    if s != ename and v > seen.get(s, 0):
                        seen[s] = v
                        engine.wait_ge(self.sem[s], v)
                for s, v in dfinal.items():
                    if v > seen.get(s, 0):
                        seen[s] = v
                        engine.wait_ge(self.sem[s], v)
                self.q[ename] = []

            block.sync(lambda e: run('sp', e))
            block.tensor(lambda e: run('pe', e))
            block.scalar(lambda e: run('act', e))
            block.vector(lambda e: run('dve', e))
            block.gpsimd(lambda e: run('pool', e))
        self.recs = {}

    def mm(self, out, lhsT, rhs, start=True, stop=True):
        self.op('pe', lambda e: e.matmul(out, lhsT, rhs, start=start, stop=stop, skip_group_check=True),
                [lhsT, rhs], [out])

    def tr(self, out, in_, ident):
        self.op('pe', lambda e: e.transpose(out, in_, ident), [in_, ident], [out])

    def act(self, out, in_, func, bias=0.0, scale=1.0, accum=None, eng='act'):
        kw = {}
        if accum is not None:
            kw['accum_out'] = accum
        self.op('act', lambda e: e.activation(out, in_, func, bias=bias, scale=scale, **kw),
                [in_, bias, scale], [out, accum])

    def tt(self, eng, out, a, b, op):
        self.op(eng, lambda e: e.tensor_tensor(out, a, b, op), [a, b], [out])

    def ts(self, eng, out, a, s1, s2, op0, op1=None):
        if op1 is None:
            self.op(eng, lambda e: e.tensor_scalar(out, a, s1, None, op0), [a, s1], [out])
        else:
            self.op(eng, lambda e: e.tensor_scalar(out, a, s1, s2, op0, op1), [a, s1, s2], [out])

    def stt(self, eng, out, a, s, b, op0, op1):
        eng = 'dve'
        self.op(eng, lambda e: e.scalar_tensor_tensor(out, a, s, b, op0, op1), [a, s, b], [out])

    def copy(self, eng, out, in_):
        if eng == 'act':
            self.op('act', lambda e: e.copy(out, in_), [in_], [out])
        else:
            self.op(eng, lambda e: e.tensor_copy(out, in_), [in_], [out])

    def memset(self, eng, ap, val):
        self.op(eng, lambda e: e.memset(ap, val), [], [ap])

    def recip(self, out, in_):
        self.op('dve', lambda e: e.reciprocal(out, in_), [in_], [out])


class Ctx:
    pass


_UID = [0]


def uname(n):
    _UID[0] += 1
    return "%s_u%d" % (n, _UID[0])


def dkey(name, r0, r1, c0, c1):
    return (name, r0, r1, c0, c1)


def build_program(S, depth, dbg=False, stub=False, only=('nsa', 'rw', 'ml')):
    NT = S // 128
    NCH = S // 512
    nc = bass.Bass("TRN2", target_bir_lowering=False)
    K = Ctx()
    K.nc, K.S, K.NT, K.NCH, K.depth = nc, S, NT, NCH, depth
    K.stub = stub
    K.only = only

    def din(name, shape, dt=F32):
        return nc.dram_tensor(name, list(shape), dt, kind="ExternalInput").ap()

    L = depth
    K.x = din("x", [S, D])
    K.c = din("c", [128, 8])
    K.w_mod = din("w_mod", [L, D, 6 * D])
    K.b_mod = din("b_mod", [L, 6 * D])
    K.gvec = {n: din(n, [L, D]) for n in ("g_pre_mix", "g_post_mix", "g_pre_ffn", "g_post_ffn")}
    K.w_in = din("w_in", [L, D, DIN])
    K.w_out = din("w_out", [L, D, D])
    K.ffn_g = din("ffn_w_gate", [L, D, DFF])
    K.ffn_u = din("ffn_w_up", [L, D, DFF])
    K.ffn_d = din("ffn_w_down", [L, DFF, D])
    K.ident = din("ident", [128, 128])
    K.tri_in = din("tri_le", [128, 128])
    ncmp = S // 16 - 1
    NN = (ncmp + 127) // 128
    NS = S // 64
    K.c_cmpb = din("c_cmpb", [NN, 128, S])
    K.c_E = din("c_E", [NS, S])
    K.c_caus = din("c_caus", [128, 128])
    K.c_wlow = din("c_wlow", [128, 128])
    K.c_vm = din("c_vm", [S, NS])
    K.c_am = din("c_am", [S, NS])
    K.c_ov = din("c_ov", [NN, 128, NS])
    K.nsa_gb = din("nsa_gate_b", [L, 12])
    K.nsa_og = din("nsa_out_g", [L, 256])
    K.nsa_w1 = [din("nsa_w1k", [L, 64, 32, 64]), din("nsa_w1v", [L, 64, 32, 64])]
    K.nsa_w2 = [din("nsa_ck_w2", [L, 64, 64]), din("nsa_cv_w2", [L, 64, 64])]
    K.nsa_pe = [din("nsa_pekT", [L, 64, 32]), din("nsa_pevT", [L, 64, 32])]
    K.rw_mua = din("rw_mua", [L, 64, 20])
    K.rw_mug = din("rw_mug", [L, 128, 1])
    K.rw_vec = din("rw_vec", [L, 64, 7, 6])
    K.rw2_mua = din("rw2_mua", [L, 128, 10])
    K.rw2_vec = din("rw2_vec", [L, 128, 7, 3])
    K.rw_w2 = din("rw_w2", [L, 64, 384])
    K.rw_a2 = din("rw_a2", [L, 64, 384])
    K.rw_g2 = din("rw_g2", [L, 128, 384])
    K.c_rmsk = din("c_rmsk", [64, 3 * 8 * 64])
    K.c_itile = din("c_itile", [64, 512])
    K.ml_cw = din("ml_cw", [L, 64, 12, 4])
    K.ml_cb = din("ml_cb", [L, 64, 12])
    K.ml_gb = din("ml_gb", [L, 12])
    K.ml_ng = din("ml_norm_g", [L, 384])
    K.out = nc.dram_tensor("out", [S, D], F32, kind="ExternalOutput").ap()
    okind = "ExternalOutput" if dbg else "Internal"
    K.PT = nc.dram_tensor("PT", [DIN, S], F32, kind=okind).ap()
    K.MIXT = nc.dram_tensor("MIXT", [D, S], F32, kind=okind).ap()
    K.X1 = nc.dram_tensor("X1", [S, D], F32, kind="Internal").ap()
    K.X2 = nc.dram_tensor("X2", [S, D], F32, kind="Internal").ap()

    with ExitStack() as gst:
        P = Prog(nc, gst)
        K.P = P
        sbg = lambda n, s, d: gst.enter_context(nc.sbuf_tensor(uname(n), list(s), d))
        K.ps = [gst.enter_context(nc.psum_tensor("psb%d" % i, [128, 512], F32)) for i in range(6)]
        K.psT = [gst.enter_context(nc.psum_tensor("psT%d" % i, [128, 1024], BF16)) for i in range(2)]
        K.psn = 0
        K.psTn = 0
        K.identf = sbg("identf", [128, 128], F32)
        K.identb = sbg("identb", [128, 128], BF16)
        K.ones1 = sbg("ones1", [1, 128], F32)
        K.MODROW = nc.dram_tensor("MODROW", [L, 6 * D], F32, kind="Internal").ap()
        K.trif = sbg("trif", [128, 128], F32)
        K.onesf = sbg("onesf", [128, 128], F32)
        P.dma('sp', K.trif[:], K.tri_in, reads=[])
        P.memset('pool', K.onesf[:], 1.0)
        K.c_itile_sb = sbg("itile_sb", [64, 512], F32)
        P.dma('sp', K.c_itile_sb[:], K.c_itile, reads=[])
        P.dma('sp', K.identf[:], K.ident, reads=[])
        P.copy('dve', K.identb[:], K.identf[:])
        P.memset('dve', K.ones1[:], 1.0)

        phase_mod(K)
        P.emit()
        xin = K.x
        for l in range(depth):
            phase_proj(K, l, xin)
            P.emit()
            phase_mixers(K, l)
            xmid = K.X1
            xout = K.out if l == depth - 1 else K.X2
            phase_out(K, l, xin, xmid, xout)
            P.emit()
            xin = xout
    return nc


def next_ps(K):
    p = K.ps[K.psn % 6]
    K.psn += 1
    return p


def next_psT(K):
    p = K.psT[K.psTn % 2]
    K.psTn += 1
    return p


def phase_mod(K):
    nc, P = K.nc, K.P
    with ExitStack() as st:
        sb = lambda n, s, d: st.enter_context(nc.sbuf_tensor(uname(n), list(s), d))
        ct = sb("m_c", [128, 8], F32)
        cs = sb("m_cs", [128, 8], F32)
        wb = [sb("m_w%d" % i, [128, 8, 512], F32) for i in range(2)]
        bm = sb("m_b", [1, K.depth, 6 * D], F32)
        mrow = [sb("m_row%d" % i, [1, 512], F32) for i in range(2)]
        P.dma('sp', ct[:], K.c, reads=[])
        P.dma('sp', bm[:], K.b_mod.rearrange("(o l) n -> o l n", o=1), reads=[])
        P.act(cs[:], ct[:], AF.Silu)
        i = 0
        for l in range(K.depth):
            for cb in range(12):
                w = wb[i % 2]
                i += 1
                src = K.w_mod[l, :, cb * 512:(cb + 1) * 512].rearrange("(k p) n -> p k n", p=128)
                P.dma('sp' if i % 2 else 'pool', w[:], src, reads=[])
                ps = next_ps(K)
                for k in range(8):
                    P.mm(ps[0:1, :], cs[:, k:k + 1], w[:, k, :], start=(k == 0), stop=(k == 7))
                mr = mrow[i % 2]
                P.tt('dve', mr[0:1, :], ps[0:1, :], bm[0:1, l, cb * 512:(cb + 1) * 512], ALU.add)
                P.dma('sp', K.MODROW[l:l + 1, cb * 512:(cb + 1) * 512], mr[0:1, :],
                      writes=[dkey('MODROW', l, l + 1, cb * 512, (cb + 1) * 512)])
        P.emit()


def phase_bcast(K, l, st, pieces):
    nc, P = K.nc, K.P
    sb = lambda n, s, d: st.enter_context(nc.sbuf_tensor(uname(n), list(s), d))
    K.bc = {}
    g = sb("b_g", [128, D], F32)
    names = ("g_pre_mix", "g_post_mix", "g_pre_ffn", "g_post_ffn")
    plan = [(0, 1, 'copy', None), (1, 0, 'scale', 0), (2, 2, 'mul', 1),
            (3, 4, 'copy', None), (4, 3, 'scale', 2), (5, 5, 'mul', 3)]
    for piece, dst, kind, gi in plan:
        if dst not in pieces:
            continue
        o = sb("bcast%d" % dst, [128, D], F32)
        K.bc[dst] = o
        P.dma('sp', o[:], K.MODROW[l:l + 1, piece * D:(piece + 1) * D].partition_broadcast(128),
              reads=[dkey('MODROW', l, l + 1, piece * D, (piece + 1) * D)])
        if gi is not None:
            P.dma('sp', g[:], K.gvec[names[gi]][l:l + 1, :].partition_broadcast(128), reads=[])
        if kind == 'scale':
            P.stt('dve', o[:], o[:], 1.0, g[:], ALU.add, ALU.mult)
        elif kind == 'mul':
            P.tt('dve', o[:], o[:], g[:], ALU.mult)


def rms_rstd(P, ss, n, eps, rstd):
    P.act(rstd, ss, AF.Sqrt, bias=float(eps), scale=1.0 / n)
    P.recip(rstd, rstd)


def make_stg(K, st, tag, n=2):
    return [st.enter_context(K.nc.sbuf_tensor(uname("%s_stg%d" % (tag, i)), [128, 1024], F32)) for i in range(n)]


def gen_load_cast_weight(K, wb, src3, kparts, ncols, stg, CW, ctr):
    P = K.P
    for k in range(kparts):
        P.dma('pool', wb[:, k, :], src3[k * 128:(k + 1) * 128, :], reads=[])
        yield


def load_cast_weight(K, st, name, src3, kparts, ncols, stg=None, CW=1024):
    nc, P = K.nc, K.P
    wb = st.enter_context(nc.sbuf_tensor(uname(name), [128, kparts, ncols], BF16))
    for _ in gen_load_cast_weight(K, wb, src3, kparts, ncols, None, CW, [0]):
        pass
    return wb


def norm_tile_to_hT(K, st_bufs, xt, bcA, bcB, hT, col0):
    P = K.P
    junk, ss, rstd, tmp, hb = st_bufs
    P.act(junk[:], xt, AF.Square, accum=ss[:, 0:1])
    rms_rstd(P, ss[:, 0:1], D, 1e-6, rstd[:, 0:1])
    P.stt('dve', tmp[:], xt, rstd[:, 0:1], bcA, ALU.mult, ALU.mult)
    P.tt('pool', hb[:], tmp[:], bcB, ALU.add)
    transpose_to(K, hb, hT, col0)


def transpose_to(K, hb, hT, col0):
    P = K.P
    psb = next_psT(K)
    for k in range(8):
        P.tr(psb[:, k * 128:(k + 1) * 128], hb[:, k * 128:(k + 1) * 128], K.identb[:])
    P.copy('act', hT[:, :, col0:col0 + 128], psb[:].rearrange("p (k t) -> p k t", k=8))


def phase_proj(K, l, xin):
    nc, P, S = K.nc, K.P, K.S
    with ExitStack() as st:
        sb = lambda n, s, d: st.enter_context(nc.sbuf_tensor(uname(n), list(s), d))
        phase_bcast(K, l, st, (0, 1))
        wb = load_cast_weight(K, st, "winb", K.w_in[l], 8, DIN)
        xts = [sb("a_x%d" % i, [128, D], F32) for i in range(2)]
        junk = sb("a_junk", [128, D], BF16)
        ss = sb("a_ss", [128, 1], F32)
        rstd = sb("a_rstd", [128, 1], F32)
        tmp = sb("a_tmp", [128, D], F32)
        hb = sb("a_hb", [128, D], BF16)
        hTs = [sb("a_hT%d" % i, [128, 8, 512], BF16) for i in range(2)]
        pos = [sb("a_po%d" % i, [128, 512], F32) for i in range(4)]
        tiles = [(c0, min(c0 + 128, DIN)) for c0 in range(0, DIN, 128)]
        ti = 0
        oi = 0
        for ch in range(K.NCH):
            hT = hTs[ch % 2]
            for t4 in range(4):
                tok0 = ch * 512 + t4 * 128
                xt = xts[ti % 2]
                ti += 1
                P.dma('sp', xt[:], xin[tok0:tok0 + 128, :], reads=[dkey(xin.tensor.name, tok0, tok0 + 128, 0, D)])
                norm_tile_to_hT(K, (junk, ss, rstd, tmp, hb), xt[:], K.bc[0][:], K.bc[1][:], hT, t4 * 128)
            for (c0, c1) in tiles:
                m = c1 - c0
                ps = next_ps(K)
                for k in range(8):
                    P.mm(ps[0:m, :], wb[:, k, c0:c1], hT[:, k, :], start=(k == 0), stop=(k == 7))
                po = pos[oi % 4]
                P.copy(('act', 'dve')[oi % 2], po[0:m, :], ps[0:m, :])
                P.dma('pool', K.PT[c0:c1, ch * 512:(ch + 1) * 512], po[0:m, :],
                      writes=[dkey('PT', c0, c1, ch * 512, (ch + 1) * 512)])
                oi += 1
        P.emit()


def phase_mixers(K, l):
    P = K.P
    if K.stub:
        for k in range(8):
            P.dma('sp', K.MIXT[k * 128:(k + 1) * 128, :], K.PT[k * 128:(k + 1) * 128, :],
                  reads=[dkey('PT', k * 128, (k + 1) * 128, 0, K.S)], writes=[dkey('MIXT', k * 128, (k + 1) * 128, 0, K.S)])
        P.emit()
        return
    if 'nsa' in K.only:
        phase_nsa(K, l)
    with ExitStack() as st:
        gens = []
        if 'rw' in K.only:
            gens.append(gen_rwkv2(K, l, st))
        if 'ml' in K.only:
            gens.append(gen_mlstm(K, l, st))
        run_gens(gens)
        P.emit()


def phase_out(K, l, xin, xmid, xout):
    nc, P, S = K.nc, K.P, K.S
    ost_ = ExitStack()
    sbo = lambda n, s, d: ost_.enter_context(nc.sbuf_tensor(uname(n), list(s), d))
    fstg = None
    wg = sbo("wgb", [128, 8, DFF], BF16)
    wu = sbo("wub", [128, 8, DFF], BF16)
    wd = sbo("wdb", [128, 22, D], BF16)
    ctr = [0]

    def chain():
        yield from gen_load_cast_weight(K, wg, K.ffn_g[l], 8, DFF, fstg, 512, ctr)
        yield from gen_load_cast_weight(K, wu, K.ffn_u[l], 8, DFF, fstg, 512, ctr)
        yield from gen_load_cast_weight(K, wd, K.ffn_d[l], 22, D, fstg, 512, ctr)
    wload = chain()

    def pull(n):
        for _ in range(n):
            try:
                next(wload)
            except StopIteration:
                return
    with ExitStack() as st:
        sb = lambda n, s, d: st.enter_context(nc.sbuf_tensor(uname(n), list(s), d))
        phase_bcast(K, l, st, (2,))
        wo = load_cast_weight(K, st, "woutb", K.w_out[l], 8, D)
        pull(1000)
        mT = [sb("o_mT%d" % i, [128, 8, 512], BF16) for i in range(2)]
        xts = [sb("o_x%d" % i, [128, D], F32) for i in range(1)]
        ys = [sb("o_y%d" % i, [128, D], F32) for i in range(2)]
        junk = sb("o_junk", [128, D], BF16)
        ss = sb("o_ss", [128, 1], F32)
        rstd = sb("o_rstd", [128, 1], F32)
        i = 0
        ti = 0

        def load_mix(ch):
            m = mT[ch % 2]
            for k in range(8):
                P.dma('pool', m[:, k, :], K.MIXT[k * 128:(k + 1) * 128, ch * 512:(ch + 1) * 512],
                      reads=[dkey('MIXT', k * 128, (k + 1) * 128, ch * 512, (ch + 1) * 512)])
        load_mix(0)
        for ch in range(K.NCH):
            m = mT[ch % 2]
            if ch + 1 < K.NCH:
                load_mix(ch + 1)
            for t4 in range(4):
                tok0 = ch * 512 + t4 * 128
                xt = xts[0]
                y = ys[ti % 2]
                ti += 1
                P.dma('sp', xt[:], xin[tok0:tok0 + 128, :], reads=[dkey(xin.tensor.name, tok0, tok0 + 128, 0, D)])
                for half in range(2):
                    ps = next_ps(K)
                    for k in range(8):
                        P.mm(ps[:, :], m[:, k, t4 * 128:(t4 + 1) * 128], wo[:, k, half * 512:(half + 1) * 512],
                             start=(k == 0), stop=(k == 7))
                    P.copy('act', y[:, half * 512:(half + 1) * 512], ps[:, :])
                P.act(junk[:], y[:], AF.Square, accum=ss[:, 0:1])
                rms_rstd(P, ss[:, 0:1], D, 1e-6, rstd[:, 0:1])
                P.stt('dve', y[:], y[:], rstd[:, 0:1], K.bc[2][:], ALU.mult, ALU.mult)
                P.tt('pool', y[:], y[:], xt[:], ALU.add)
                P.dma('pool', xmid[tok0:tok0 + 128, :], y[:], writes=[dkey(xmid.tensor.name, tok0, tok0 + 128, 0, D)])
        pull(1000)
        P.emit()
    with ExitStack() as st:
        sb = lambda n, s, d: st.enter_context(nc.sbuf_tensor(uname(n), list(s), d))
        phase_bcast(K, l, st, (3, 4, 5))
        xt = sb("f_x", [128, D], F32)
        junk = sb("f_junk", [128, D], BF16)
        ss = sb("f_ss", [128, 1], F32)
        rstd = sb("f_rstd", [128, 1], F32)
        hb = sb("f_hb", [128, D], BF16)
        hT = sb("f_hT", [128, 8, 512], BF16)
        sg = sb("f_sg", [128, 512], F32)
        aT = sb("f_aT", [128, 22, 512], BF16)
        y = sb("f_y", [128, D], F32)
        tmp = y
        for ch in range(K.NCH):
            for t4 in range(4):
                tok0 = ch * 512 + t4 * 128
                P.dma('sp', xt[:], xmid[tok0:tok0 + 128, :], reads=[dkey(xmid.tensor.name, tok0, tok0 + 128, 0, D)])
                norm_tile_to_hT(K, (junk, ss, rstd, tmp, hb), xt[:], K.bc[3][:], K.bc[4][:], hT, t4 * 128)
            for f in range(22):
                pg = next_ps(K)
                pu = next_ps(K)
                for k in range(8):
                    P.mm(pg[:, :], wg[:, k, f * 128:(f + 1) * 128], hT[:, k, :], start=(k == 0), stop=(k == 7))
                for k in range(8):
                    P.mm(pu[:, :], wu[:, k, f * 128:(f + 1) * 128], hT[:, k, :], start=(k == 0), stop=(k == 7))
                P.act(sg[:], pg[:, :], AF.Silu)
                P.tt('dve', aT[:, f, :], sg[:], pu[:, :], ALU.mult)
            for t4 in range(4):
                tok0 = ch * 512 + t4 * 128
                P.dma('sp', xt[:], xmid[tok0:tok0 + 128, :], reads=[dkey(xmid.tensor.name, tok0, tok0 + 128, 0, D)])
                for half in range(2):
                    ps = next_ps(K)
                    for f in range(22):
                        P.mm(ps[:, :], aT[:, f, t4 * 128:(t4 + 1) * 128], wd[:, f, half * 512:(half + 1) * 512], start=(f == 0), stop=(f == 21))
                    P.copy('act', y[:, half * 512:(half + 1) * 512], ps[:, :])
                P.act(junk[:], y[:], AF.Square, accum=ss[:, 0:1])
                rms_rstd(P, ss[:, 0:1], D, 1e-6, rstd[:, 0:1])
                P.stt('dve', y[:], y[:], rstd[:, 0:1], K.bc[5][:], ALU.mult, ALU.mult)
                P.tt('pool', y[:], y[:], xt[:], ALU.add)
                P.dma('pool', xout[tok0:tok0 + 128, :], y[:], writes=[dkey(xout.tensor.name, tok0, tok0 + 128, 0, D)])
        P.emit()
    ost_.close()


def make_in_maps(inputs, S, depth, nb):
    x = np.asarray(inputs["x"], np.float32)
    c = np.asarray(inputs["c"], np.float32)
    shared = {}
    for n in ("w_mod", "b_mod", "g_pre_mix", "g_post_mix", "g_pre_ffn", "g_post_ffn", "w_in", "w_out",
              "ffn_w_gate", "ffn_w_up", "ffn_w_down"):
        shared[n] = np.ascontiguousarray(np.asarray(inputs[n], np.float32)[:depth])
    shared["ident"] = np.eye(128, dtype=np.float32)
    shared["tri_le"] = np.triu(np.ones((128, 128), np.float32))
    f = lambda n: np.asarray(inputs[n], np.float32)[:depth]
    ncmp = S // 16 - 1
    NN = (ncmp + 127) // 128
    NS = S // 64
    n_ = np.arange(NN * 128)[:, None]
    q_ = np.arange(S)[None, :]
    cm = np.where((16 * n_ + 31 <= q_) & (n_ < ncmp), 0.0, NEGB).astype(np.float32)
    shared["c_cmpb"] = np.ascontiguousarray(cm.reshape(NN, 128, S))
    shared["c_E"] = (np.arange(S)[None, :] // 64 == np.arange(NS)[:, None]).astype(np.float32)
    k_ = np.arange(128)[:, None]
    qq = np.arange(128)[None, :]
    shared["c_caus"] = np.where(k_ <= qq, 0.0, NEGB).astype(np.float32)
    shared["c_wlow"] = np.where(k_ > qq, 0.0, NEGB).astype(np.float32)
    pos = np.arange(S)[:, None]
    j_ = np.arange(NS)[None, :]
    cur = pos // 64
    forced = (j_ == 0) | (j_ == cur) | (j_ == cur - 1)
    valid = j_ <= cur
    shared["c_vm"] = (valid & ~forced).astype(np.float32)
    shared["c_am"] = np.where(forced, 1000.0 + j_, np.where(valid, 0.0, -1.0 - j_)).astype(np.float32)
    cs_ = (np.arange(NN * 128) * 16)[:, None]
    ss_ = (np.arange(NS) * 64)[None, :]
    ov = ((cs_ < ss_ + 64) & (cs_ + 32 > ss_) & (np.arange(NN * 128)[:, None] < ncmp)).astype(np.float32)
    shared["c_ov"] = np.ascontiguousarray(ov.reshape(NN, 128, NS))
    mu = f("rw_mu")
    mua = np.concatenate([mu[:, :1152].reshape(depth, 18, 64), mu[:, 1152:1280].reshape(depth, 2, 64)], axis=1)
    shared["rw_mua"] = np.ascontiguousarray(mua.transpose(0, 2, 1))
    shared["rw_mug"] = np.ascontiguousarray(mu[:, 1280:1408].reshape(depth, 128, 1))
    vecs = [f(n).reshape(depth, 6, 64) for n in ("rw_w0", "rw_a0", "rw_kk", "rw_ka", "rw_ln_w", "rw_ln_b")] + [f("rw_rk")]
    shared["rw_vec"] = np.ascontiguousarray(np.stack(vecs, axis=1).transpose(0, 3, 1, 2))
    m2 = np.zeros((depth, 128, 10), np.float32)
    for q in range(3):
        for h in range(6):
            g, hh = divmod(h, 3)
            m2[:, 64 * g:64 * g + 64, q * 3 + hh] = mu[:, q * 384 + h * 64:q * 384 + (h + 1) * 64]
    m2[:, 0:64, 9] = mu[:, 1152:1216]
    m2[:, 64:128, 9] = mu[:, 1216:1280]
    shared["rw2_mua"] = m2
    v7 = np.stack(vecs, axis=1)
    shared["rw2_vec"] = np.ascontiguousarray(v7.reshape(depth, 7, 2, 3, 64).transpose(0, 2, 4, 1, 3).reshape(depth, 128, 7, 3))
    shared["rw_w2"] = f("rw_w2")
    shared["rw_a2"] = f("rw_a2")
    shared["rw_g2"] = f("rw_g2")
    s_ = np.arange(64)[:, None]
    t_ = np.arange(64)[None, :]
    m3 = np.stack([(s_ < t_), (s_ <= t_), (s_ > t_)]).astype(np.float32)
    shared["c_rmsk"] = np.ascontiguousarray(np.tile(m3.transpose(1, 0, 2)[:, :, None, :], (1, 1, 8, 1)).reshape(64, 3 * 8 * 64))
    shared["c_itile"] = np.ascontiguousarray(np.tile(np.eye(64, dtype=np.float32)[:, None, :], (1, 8, 1)).reshape(64, 512))
    shared["nsa_gate_b"] = f("nsa_gate_b")
    shared["nsa_out_g"] = f("nsa_out_g")
    shared["nsa_w1k"] = np.ascontiguousarray(f("nsa_ck_w1").reshape(depth, 32, 64, 64).transpose(0, 2, 1, 3))
    shared["nsa_w1v"] = np.ascontiguousarray(f("nsa_cv_w1").reshape(depth, 32, 64, 64).transpose(0, 2, 1, 3))
    shared["nsa_ck_w2"] = f("nsa_ck_w2")
    shared["nsa_cv_w2"] = f("nsa_cv_w2")
    shared["nsa_pekT"] = np.ascontiguousarray(f("nsa_pe_k").transpose(0, 2, 1))
    shared["nsa_pevT"] = np.ascontiguousarray(f("nsa_pe_v").transpose(0, 2, 1))
    cw = f("ml_conv_w")
    shared["ml_cw"] = np.ascontiguousarray(cw.transpose(0, 2, 1).reshape(depth, 12, 64, 4).transpose(0, 2, 1, 3))
    shared["ml_cb"] = np.ascontiguousarray(f("ml_conv_b").reshape(depth, 12, 64).transpose(0, 2, 1))
    shared["ml_gb"] = np.ascontiguousarray(np.concatenate([f("ml_ig_b"), f("ml_fg_b")], axis=1))
    shared["ml_norm_g"] = np.ascontiguousarray(f("ml_norm_g"))
    maps = []
    for b in range(nb):
        m = dict(shared)
        m["x"] = np.ascontiguousarray(x[b, :S])
        m["c"] = np.ascontiguousarray(c[b].reshape(8, 128).T)
        maps.append(m)
    return maps


_CACHE = {}


def kernel(**inputs):
    S, depth, nb = 4096, 2, 8
    if "prog" not in _CACHE:
        _CACHE["prog"] = build_program(S, depth)
    nc = _CACHE["prog"]
    maps = make_in_maps(inputs, S, depth, nb)
    res = run_bass_kernel_spmd(nc, maps, core_ids=list(range(nb)))
    return np.stack([np.asarray(r["out"], np.float32) for r in res.results], axis=0)


ML0 = 652 + 1408


def bl(ap2, n):
    p, m = ap2.shape
    return ap2.unsqueeze(2).to_broadcast([p, m, n])


ML_MS = 256


def gen_mlstm(K, l, st):
    nc, P, S = K.nc, K.P, K.S
    MS = ML_MS
    MC = MS // 128
    if True:
        sb = lambda n, s, d: st.enter_context(nc.sbuf_tensor(uname(n), list(s), d))
        cw = sb("ml_cw", [64, 12, 4], F32)
        cb = sb("ml_cb", [64, 12], F32)
        gb = sb("ml_gb", [128, 12], F32)
        ng = sb("ml_ng", [128, 384], F32)
        P.dma('sp', cw[:], K.ml_cw[l], reads=[])
        P.dma('sp', cb[:], K.ml_cb[l], reads=[])
        P.dma('sp', gb[:], K.ml_gb[l:l + 1, :].partition_broadcast(128), reads=[])
        P.dma('sp', ng[:], K.ml_ng[l:l + 1, :].partition_broadcast(128), reads=[])
        xin = [sb("ml_xin%d" % i, [64, 12, MS + 3], F32) for i in range(1)]
        xb = sb("ml_xb", [64, 12, MS + 3], BF16)
        Dg = sb("ml_Dg", [64, 12, 4, 64], BF16)
        for idx in range(12):
            for j in range(4):
                P.act(Dg[:, idx, j, :], K.identf[0:64, 0:64], AF.Copy, scale=cw[:, idx, j:j + 1])
        qkT = sb("ml_qkT", [64, 12, MS], BF16)
        vin = sb("ml_vin", [128, 3, MS], F32)
        vinb = sb("ml_vinb", [128, 3, MS], BF16)
        V1 = sb("ml_V1", [128, MC, 6, 65], BF16)
        oin = sb("ml_oin", [128, 3, MS], F32)
        OG = sb("ml_OG", [128, MC, 384], F32)
        gin = sb("ml_gin", [12, MS], F32)
        gsb = sb("ml_gsb", [128, MC, 12], F32)
        lf = sb("ml_lf", [128, MC, 6], F32)
        es = sb("ml_es", [128, MC, 6], F32)
        eb = sb("ml_eb", [128, MC, 6], F32)
        eg = sb("ml_eg", [128, MC, 6], F32)
        eh = sb("ml_eh", [128, MC, 6], F32)
        PsT = [sb("ml_PsT%d" % i, [128, 6, 128], BF16) for i in range(2)]
        Kh = [sb("ml_Kh%d" % i, [128, 6, 64], BF16) for i in range(2)]
        CT = sb("ml_CT", [64, 6, 65], F32)
        CTb = sb("ml_CTb", [64, 6, 65], BF16)
        dd = sb("ml_dd", [128, 6], F32)
        rr = sb("ml_rr", [128, 6], F32)
        hh = sb("ml_hh", [128, 6, 64], F32)
        sq = sb("ml_sq", [128, 6, 64], F32)
        ssum = sb("ml_ssum", [128, 6], F32)
        ho = sb("ml_ho", [128, 384], F32)
        ost = [sb("ml_ost%d" % i, [128, 3, MS], F32) for i in range(1)]
        P.memset('dve', CT[:], 0.0)
        P.memset('dve', CTb[:], 0.0)
        P.memset('pool', V1[:], 1.0)
        NSCm = S // MS
        for sc in range(NSCm):
            t0 = sc * MS
            xi = xin[0]
            for idx in range(12):
                r0 = ML0 + idx * 64
                if sc == 0:
                    P.memset('pool', xi[:, idx, 0:3], 0.0)
                    P.dma('sp', xi[:, idx, 3:MS + 3], K.PT[r0:r0 + 64, 0:MS], reads=[dkey('PT', r0, r0 + 64, 0, MS)])
                else:
                    P.dma('sp', xi[:, idx, :], K.PT[r0:r0 + 64, t0 - 3:t0 + MS], reads=[dkey('PT', r0, r0 + 64, t0 - 3, t0 + MS)])
            for i in range(3):
                r0 = ML0 + 768 + i * 128
                P.dma('pool', vin[:, i, :], K.PT[r0:r0 + 128, t0:t0 + MS], reads=[dkey('PT', r0, r0 + 128, t0, t0 + MS)])
                r0 = ML0 + 1152 + i * 128
                P.dma('pool', oin[:, i, :], K.PT[r0:r0 + 128, t0:t0 + MS], reads=[dkey('PT', r0, r0 + 128, t0, t0 + MS)])
            r0 = ML0 + 1536
            P.dma('sp', gin[:], K.PT[r0:r0 + 12, t0:t0 + MS], reads=[dkey('PT', r0, r0 + 12, t0, t0 + MS)])
            P.copy('act', xb[:], xi[:])
            for idx in range(12):
                ps = next_ps(K)
                for j in range(4):
                    P.mm(ps[0:64, 0:MS], Dg[:, idx, j, :], xb[:, idx, j:j + MS], start=(j == 0), stop=(j == 3))
                P.act(qkT[:, idx, :], ps[0:64, 0:MS], AF.Silu, bias=cb[:, idx:idx + 1])
            yield (sc + 0.1) / NSCm
            P.copy('pool', vinb[:], vin[:])
            P.act(oin[:], oin[:], AF.Sigmoid)
            for c in range(MC):
                pt = next_psT(K)
                for i in range(3):
                    P.tr(pt[:, i * 128:(i + 1) * 128], vinb[:, i, c * 128:(c + 1) * 128], K.identb[:])
                P.copy('act', V1[:, c, :, 0:64], pt[:, 0:384].rearrange("p (h e) -> p h e", h=6))
                ps = next_ps(K)
                for i in range(3):
                    P.tr(ps[:, i * 128:(i + 1) * 128], oin[:, i, c * 128:(c + 1) * 128], K.identf[:])
                P.copy('dve', OG[:, c, :], ps[:, 0:384])
                ps = next_ps(K)
                P.tr(ps[:, 0:12], gin[:, c * 128:(c + 1) * 128], K.identf[0:12, 0:12])
                P.tt('dve', gsb[:, c, :], ps[:, 0:12], gb[:], ALU.add)
            P.act(lf[:], gsb[:, :, 6:12], AF.Exp, scale=-1.0)
            P.act(lf[:], lf[:], AF.Ln, bias=1.0)
            P.ts('dve', lf[:], lf[:], -1.0, None, ALU.mult)
            for c in range(MC):
                ps = next_ps(K)
                P.mm(ps[:, 0:6], K.trif[:], lf[:, c, :])
                P.mm(ps[:, 8:14], K.onesf[:], lf[:, c, :], start=False)
                P.act(eb[:, c, :], ps[:, 0:6], AF.Exp)
                P.act(eg[:, c, :], ps[:, 8:14], AF.Exp)
                P.tt('dve', es[:, c, :], gsb[:, c, 0:6], ps[:, 0:6], ALU.subtract)
                P.act(es[:, c, :], es[:, c, :], AF.Exp, bias=-2.0794415416798357)
                P.tt('dve', eh[:, c, :], es[:, c, :], eg[:, c, :], ALU.mult)
            yield (sc + 0.2) / NSCm
            for c in range(MC):
                cs_ = slice(c * 128, (c + 1) * 128)
                pst = PsT[c % 2]
                kh = Kh[c % 2]
                psA = next_ps(K)
                psB = next_ps(K)
                for h in range(6):
                    pp = psA if h < 4 else psB
                    o = (h % 4) * 128
                    P.mm(pp[:, o:o + 128], qkT[:, 6 + h, cs_], qkT[:, h, cs_], start=(h % 4 == 0))
                for h in range(6):
                    pp = psA if h < 4 else psB
                    o = (h % 4) * 128
                    P.stt(('dve', 'pool')[h % 2] if False else 'dve', pst[:, h, :], pp[:, o:o + 128], es[:, c, h:h + 1], K.trif[:], ALU.mult, ALU.mult)
                pt = next_psT(K)
                for h in range(6):
                    P.tr(pt[:, h * 64:(h + 1) * 64], qkT[:, 6 + h, cs_], K.identb[0:64, 0:64])
                P.tt('dve', kh[:], pt[:, 0:384].rearrange("p (h e) -> p h e", h=6), bl(eh[:, c, :], 64), ALU.mult)
                pn = next_ps(K)
                for h in range(6):
                    P.mm(pn[:, h * 65:(h + 1) * 65], pst[:, h, :], V1[:, c, h, :], start=(h == 0))
                    P.mm(pn[:, h * 65:(h + 1) * 65], qkT[:, h, cs_], CTb[:, h, :], start=False)
                pc = next_ps(K)
                for h in range(6):
                    P.mm(pc[0:64, h * 65:(h + 1) * 65], kh[:, h, :], V1[:, c, h, :], start=(h == 0))
                P.tt('dve', CT[:], CT[:], bl(eg[0:64, c, :], 65), ALU.mult)
                P.tt('dve', CT[:], CT[:], pc[0:64, 0:390].rearrange("p (h e) -> p h e", h=6), ALU.add)
                P.copy('pool', CTb[:], CT[:])
                pn3 = pn[:, 0:390].rearrange("p (h e) -> p h e", h=6)
                P.act(dd[:], pn3[:, :, 64], AF.Abs)
                P.tt('dve', dd[:], dd[:], eb[:, c, :], ALU.mult)
                P.ts('dve', dd[:], dd[:], 1.0, None, ALU.max)
                P.recip(rr[:], dd[:])
                P.tt('dve', rr[:], rr[:], eb[:, c, :], ALU.mult)
                P.tt('dve', hh[:], pn3[:, :, 0:64], bl(rr[:], 64), ALU.mult)
                P.tt('pool', sq[:], hh[:], hh[:], ALU.mult)
                P.op('dve', lambda e: e.tensor_reduce(ssum[:], sq[:], AX.X, ALU.add), [sq[:]], [ssum[:]])
                P.act(ssum[:], ssum[:], AF.Sqrt, bias=1e-6, scale=1.0 / 64)
                P.recip(ssum[:], ssum[:])
                P.tt('dve', hh[:], hh[:], bl(ssum[:], 64), ALU.mult)
                ho3 = hh[:].rearrange("p h e -> p (h e)")
                P.tt('pool', ho[:], ho3, ng[:], ALU.mult)
                P.tt('dve', ho[:], ho[:], OG[:, c, :], ALU.mult)
                po = next_ps(K)
                for i in range(3):
                    P.tr(po[:, i * 128:(i + 1) * 128], ho[:, i * 128:(i + 1) * 128], K.identf[:])
                P.copy('act', ost[0][:, :, cs_], po[:, 0:384].rearrange("p (i t) -> p i t", i=3))
                yield (sc + 0.2 + 0.8 * (c + 1) / MC) / NSCm
            for i in range(3):
                r0 = 640 + i * 128
                P.dma('sp', K.MIXT[r0:r0 + 128, t0:t0 + MS], ost[0][:, i, :], writes=[dkey('MIXT', r0, r0 + 128, t0, t0 + MS)])


def run_gens(gens):
    prog = [0.0] * len(gens)
    live = list(range(len(gens)))
    while live:
        i = min(live, key=lambda k: prog[k])
        try:
            v = next(gens[i])
            prog[i] = v if v is not None else prog[i] + 1e-3
        except StopIteration:
            live.remove(i)


def phase_mlstm(K, l):
    with ExitStack() as st:
        run_gens([gen_mlstm(K, l, st)])
        K.P.emit()


def gelu_tanh(K, out_bf, x, t1, t2):
    P = K.P
    P.tt('dve', t1, x, x, ALU.mult)
    P.ts('dve', t1, t1, 0.044715, 1.0, ALU.mult, ALU.add)
    P.tt('dve', t1, t1, x, ALU.mult)
    P.act(t2, t1, AF.Sigmoid, scale=1.5957691216057308)
    P.tt('dve', out_bf, x, t2, ALU.mult)


def phase_nsa(K, l):
    nc, P, S, NT = K.nc, K.P, K.S, K.NT
    ncmp = S // 16 - 1
    NN = (ncmp + 127) // 128
    NS = S // 64
    CW = min(S, 2048)
    with ExitStack() as st:
        sb = lambda n, s, d: st.enter_context(nc.sbuf_tensor(uname(n), list(s), d))
        QT = sb("n_QT", [64, NT, 4, 128], BF16)
        kcT = sb("n_kcT", [64, S], BF16)
        vcT = sb("n_vcT", [64, S], BF16)
        ksT = sb("n_ksT", [64, S], BF16)
        kwT = sb("n_kwT", [64, S], BF16)
        vtmp = sb("n_vtmp", [64, S], BF16)
        V1s = sb("n_V1s", [128, NT, 65], BF16)
        V1w = sb("n_V1w", [128, NT, 65], BF16)
        G = sb("n_G", [128, NT, 12], F32)
        stg = [sb("n_stg%d" % i, [128, CW], F32) for i in range(3)]
        gin = sb("n_gin", [12, S], F32)
        gbias = sb("n_gbias", [128, 12], F32)
        gout = sb("n_gout", [128, 256], F32)
        cmpb = sb("n_cmpb", [128, NN, S], BF16)
        Eb = sb("n_Eb", [NS, S], BF16)
        causb = sb("n_causb", [128, 128], BF16)
        wlowb = sb("n_wlowb", [128, 128], BF16)
        vm = sb("n_vm", [128, NT, NS], F32)
        am = sb("n_am", [128, NT, NS], F32)
        OV = sb("n_OV", [128, NN, 64], BF16)
        VC1 = sb("n_VC1", [128, NN, 65], BF16)
        KcT = sb("n_KcT", [64, NN * 128], BF16)
        si = [0]

        def load_cast(dst, src, rows, dkeys, cols, tiled=False):
            for c0 in range(0, cols, CW):
                c1 = min(cols, c0 + CW)
                s_ = stg[si[0] % 3]
                si[0] += 1
                P.dma(('sp', 'pool')[si[0] % 2], s_[0:rows, 0:c1 - c0], src[:, c0:c1],
                      reads=[(dkeys[0], dkeys[1], dkeys[2], c0, c1)] if dkeys else [])
                if tiled:
                    P.copy(('dve', 'act')[si[0] % 2], dst[:, c0 // 128:c1 // 128, :], s_[0:rows, 0:c1 - c0].rearrange("p (t q) -> p t q", q=128))
                else:
                    P.copy(('dve', 'act')[si[0] % 2], dst[:, c0:c1], s_[0:rows, 0:c1 - c0])

        for h in range(4):
            load_cast(QT[:, :, h, :], K.PT[h * 64:(h + 1) * 64, :], 64, ('PT', h * 64, (h + 1) * 64), S, tiled=True)
        for dst, r0 in ((kcT, 256), (vcT, 320), (ksT, 384), (kwT, 512)):
            load_cast(dst[:, :], K.PT[r0:r0 + 64, :], 64, ('PT', r0, r0 + 64), S)
        for NNi in range(NN):
            load_cast(cmpb[:, NNi, :], K.c_cmpb[NNi], 128, None, S)
        load_cast(Eb[:, :], K.c_E[0:NS, 0:S], NS, None, S)
        load_cast(causb[:, :], K.c_caus, 128, None, 128)
        load_cast(wlowb[:, :], K.c_wlow, 128, None, 128)
        for NNi in range(NN):
            load_cast(OV[:, NNi, :], K.c_ov[NNi, :, 0:NS], 128, None, NS) if False else None
        P.dma('sp', vm[:], K.c_vm.rearrange("(t p) j -> p t j", p=128), reads=[])
        P.dma('sp', am[:], K.c_am.rearrange("(t p) j -> p t j", p=128), reads=[])
        P.dma('sp', gbias[:], K.nsa_gb[l:l + 1, :].partition_broadcast(128), reads=[])
        P.dma('sp', gout[:], K.nsa_og[l:l + 1, :].partition_broadcast(128), reads=[])
        P.memset('pool', V1s[:], 1.0)
        P.memset('pool', V1w[:], 1.0)
        P.memset('pool', VC1[:], 1.0)
        ovs = sb("n_ovs", [128, NN, NS], F32)
        P.dma('sp', ovs[:], K.c_ov.rearrange("n p j -> p n j"), reads=[])
        OVb = sb("n_OVb", [128, NN, NS], BF16)
        P.copy('dve', OVb[:], ovs[:])
        for (Vd, r0) in ((V1s, 448), (V1w, 576)):
            load_cast(vtmp[:, :], K.PT[r0:r0 + 64, :], 64, ('PT', r0, r0 + 64), S)
            for t8 in range(0, NT, 8):
                nt_ = min(8, NT - t8)
                pt = next_psT(K)
                for j in range(nt_):
                    P.tr(pt[:, j * 64:(j + 1) * 64], vtmp[:, (t8 + j) * 128:(t8 + j + 1) * 128], K.identb[0:64, 0:64])
                P.copy('act', Vd[:, t8:t8 + nt_, 0:64], pt[:, 0:nt_ * 64].rearrange("p (t e) -> p t e", t=nt_))
        P.dma('sp', gin[:], K.PT[640:652, :], reads=[dkey('PT', 640, 652, 0, S)])
        for t0_ in range(0, NT, 32):
            ps = next_ps(K)
            n_ = min(32, NT - t0_)
            for j in range(n_):
                P.tr(ps[:, j * 12:(j + 1) * 12], gin[:, (t0_ + j) * 128:(t0_ + j + 1) * 128], K.identf[0:12, 0:12])
            P.tt('dve', G[:, t0_:t0_ + n_, :], ps[:, 0:n_ * 12].rearrange("p (t g) -> p t g", t=n_),
                 gbias[:].unsqueeze(1).to_broadcast([128, n_, 12]), ALU.add)
        P.act(G[:], G[:], AF.Sigmoid)
        w1s = sb("n_w1s", [64, 32, 64], F32)
        w1b = sb("n_w1b", [64, 32, 64], BF16)
        w2s = sb("n_w2s", [64, 64], F32)
        w2b = sb("n_w2b", [64, 64], BF16)
        pes = sb("n_pes", [64, 32], F32)
        peb = sb("n_peb", [64, 32], BF16)
        c1 = sb("n_c1", [64, 1], F32)
        hx = sb("n_hx", [64, 512], F32)
        ht1 = sb("n_ht1", [64, 512], F32)
        ht2 = sb("n_ht2", [64, 512], F32)
        hidb = sb("n_hidb", [64, NN * 128], BF16)
        for which in range(2):
            src = (kcT, vcT)[which]
            P.dma('sp', w1s[:], K.nsa_w1[which][l], reads=[])
            P.dma('sp', w2s[:], K.nsa_w2[which][l], reads=[])
            P.dma('sp', pes[:], K.nsa_pe[which][l], reads=[])
            P.copy('dve', w1b[:], w1s[:])
            P.copy('dve', w2b[:], w2s[:])
            P.copy('dve', peb[:], pes[:])
            ps = next_ps(K)
            for j in range(32):
                P.mm(ps[0:64, 0:1], w1b[:, j, :], peb[:, j:j + 1], start=(j == 0), stop=(j == 31))
            P.copy('dve', c1[:], ps[0:64, 0:1])
            ps = next_ps(K)
            for j in range(32):
                P.mm(ps[0:64, 0:ncmp], w1b[:, j, :], src[:, j:j + 16 * (ncmp - 1) + 1:16], start=(j == 0), stop=(j == 31))
            P.act(hx[:, 0:ncmp], ps[0:64, 0:ncmp], AF.Identity, bias=c1[:, 0:1])
            P.memset('pool', hidb[:], 0.0)
            gelu_tanh(K, hidb[:, 0:ncmp], hx[:, 0:ncmp], ht1[:, 0:ncmp], ht2[:, 0:ncmp])
            if which == 0:
                ps = next_ps(K)
                P.mm(ps[0:64, 0:NN * 128], w2b[:], hidb[:])
                P.copy('dve', KcT[:], ps[0:64, 0:NN * 128])
            else:
                for nn in range(NN):
                    ps = next_ps(K)
                    P.mm(ps[:, 0:64], hidb[:, nn * 128:(nn + 1) * 128], w2b[:])
                    P.copy('dve', VC1[:, nn, 0:64], ps[:, 0:64])
        Pex = [sb("n_Pex%d" % i, [128, 512], BF16) for i in range(3)]
        pxi = [0]
        rden = sb("n_rden", [128, 4], F32)
        imp = sb("n_imp", [128, NS], F32)
        mx = sb("n_mx", [128, 8], F32)
        s2 = sb("n_s2", [128, NS], F32)
        s3 = sb("n_s3", [128, NS], F32)
        selb = sb("n_selb", [128, NS], BF16)
        selT = sb("n_selT", [NS, 4, 128], BF16)
        caus4 = sb("n_caus4", [128, 4, 128], BF16)
        wlow4 = sb("n_wlow4", [128, 4, 128], BF16)
        cmp4 = [sb("n_cmp4_%d" % i, [128, 4, 128], BF16) for i in range(2)]
        P.copy('dve', caus4[:], causb[:].unsqueeze(1).to_broadcast([128, 4, 128]))
        P.copy('dve', wlow4[:], wlowb[:].unsqueeze(1).to_broadcast([128, 4, 128]))
        dn = sb("n_dn", [128, 12], F32)
        oo = sb("n_oo", [128, 4, 64], F32)
        otmp = sb("n_otmp", [128, 4, 64], F32)
        junk = sb("n_junk", [128, 256], BF16)
        ss = sb("n_ss", [128, 1], F32)
        ost = [sb("n_ost%d" % i, [128, 2, 512], F32) for i in range(2)]
        pc, pu, psel, pw = K.ps[0], K.ps[1], K.ps[2], K.ps[3]
        sci = [0]

        def score_bank():
            b = K.ps[4 + sci[0] % 2]
            sci[0] += 1
            return b

        def score(blk):
            kT, V1, kt, q0, masks, acc, first, extra = blk
            ps = score_bank()
            P.mm(ps[:], kT[:, kt * 128:(kt + 1) * 128], QT[:, q0 // 128].rearrange("p h q -> p (h q)"), start=True, stop=False)
            for (ml, mr) in masks:
                P.mm(ps[:], ml, mr.rearrange("p h q -> p (h q)"), start=False, stop=False)
            px = Pex[pxi[0] % 3]
            pxi[0] += 1
            P.act(px[:], ps[:], AF.Exp, scale=0.125)
            return px

        def pv(blk, px):
            kT, V1, kt, q0, masks, acc, first, extra = blk
            for h in range(4):
                P.mm(acc[:, h * 65:(h + 1) * 65], px[:, h * 128:(h + 1) * 128], V1[:, kt, :], start=(first and h == 0), stop=False)
            if extra is not None:
                nn, a = extra
                for h in range(4):
                    P.mm(pu[:, h * NS:(h + 1) * NS], px[:, h * 128:(h + 1) * 128], OVb[:, nn, :], start=(a == 0 and h == 0), stop=False)

        def run_blocks(blks):
            prev = None
            for blk in blks:
                px = score(blk)
                if prev is not None:
                    pv(*prev)
                prev = (blk, px)
            if prev is not None:
                pv(*prev)

        for i in range(NT):
            q0 = i * 128
            nts = [nn for nn in range(NN) if nn * 2048 + 31 <= q0 + 127]
            blks = []
            for a, nn in enumerate(nts):
                c4 = cmp4[a % 2]
                P.copy('pool', c4[:], cmpb[:, nn, q0:q0 + 128].unsqueeze(1).to_broadcast([128, 4, 128]))
                blks.append((KcT, VC1, nn, q0, [(K.identb[:], c4[:])], pc, a == 0, (nn, a)))
            run_blocks(blks)
            pc3 = pc[:, 0:260].rearrange("p (h e) -> p h e", h=4)
            P.ts('dve', rden[:], pc3[:, :, 64], 1e-30, None, ALU.max)
            P.recip(rden[:], rden[:])
            P.ts('dve', imp[:], pu[:, 0:NS], rden[:, 0:1], None, ALU.mult)
            for h in range(1, 4):
                P.stt('dve', imp[:], pu[:, h * NS:(h + 1) * NS], rden[:, h:h + 1], imp[:], ALU.mult, ALU.add)
            P.tt('dve', imp[:], imp[:], vm[:, i, :], ALU.mult)
            P.tt('dve', imp[:], imp[:], am[:, i, :], ALU.add)
            P.op('dve', lambda e: e.max(mx[:], imp[:]), [imp[:]], [mx[:]])
            P.op('dve', lambda e: e.match_replace(s2[:], mx[:], imp[:], -1e9), [mx[:], imp[:]], [s2[:]])
            P.op('dve', lambda e: e.max(mx[:], s2[:]), [s2[:]], [mx[:]])
            P.op('dve', lambda e: e.match_replace(s3[:], mx[:], s2[:], -1e9), [mx[:], s2[:]], [s3[:]])
            P.ts('dve', selb[:], s3[:], -1e8, NEGB, ALU.is_gt, ALU.mult)
            k0 = max(0, i - 4)
            blks = []
            for kt in range(k0, i + 1):
                masks = []
                if kt == i:
                    masks.append((K.identb[:], caus4[:]))
                if kt == i - 4:
                    masks.append((K.identb[:], wlow4[:]))
                blks.append((kwT, V1w, kt, q0, masks, pw, kt == k0, None))
            run_blocks(blks)
            pt = next_psT(K)
            P.tr(pt[0:NS, 0:128], selb[:], K.identb[:])
            P.copy('dve', selT[:], pt[0:NS, 0:128].unsqueeze(1).to_broadcast([NS, 4, 128]))
            blks = []
            for kt in range(i + 1):
                masks = [(Eb[:, kt * 128:(kt + 1) * 128], selT[:])]
                if kt == i:
                    masks.append((K.identb[:], caus4[:]))
                blks.append((ksT, V1s, kt, q0, masks, psel, kt == 0, None))
            run_blocks(blks)
            ps3 = psel[:, 0:260].rearrange("p (h e) -> p h e", h=4)
            pw3 = pw[:, 0:260].rearrange("p (h e) -> p h e", h=4)
            P.copy('dve', dn[:, 0:4], pc3[:, :, 64])
            P.copy('dve', dn[:, 4:8], ps3[:, :, 64])
            P.copy('dve', dn[:, 8:12], pw3[:, :, 64])
            P.ts('dve', dn[:], dn[:], 1e-30, None, ALU.max)
            P.recip(dn[:], dn[:])
            P.tt('dve', dn[:], dn[:], G[:, i, :], ALU.mult)
            P.tt('dve', oo[:], pc3[:, :, 0:64], bl(dn[:, 0:4], 64), ALU.mult)
            P.tt('dve', otmp[:], ps3[:, :, 0:64], bl(dn[:, 4:8], 64), ALU.mult)
            P.tt('pool', oo[:], oo[:], otmp[:], ALU.add)
            P.tt('dve', otmp[:], pw3[:, :, 0:64], bl(dn[:, 8:12], 64), ALU.mult)
            P.tt('pool', oo[:], oo[:], otmp[:], ALU.add)
            o2 = oo[:].rearrange("p h e -> p (h e)")
            P.act(junk[:], o2, AF.Square, accum=ss[:, 0:1])
            rms_rstd(P, ss[:, 0:1], 256, 1e-6, ss[:, 0:1])
            P.stt('dve', o2, o2, ss[:, 0:1], gout[:], ALU.mult, ALU.mult)
            po = next_psT(K) if False else None
            pso = K.ps[4 + sci[0] % 2]
            sci[0] += 1
            for j in range(2):
                P.tr(pso[:, j * 128:(j + 1) * 128], o2[:, j * 128:(j + 1) * 128], K.identf[:])
            os_ = ost[(i // 4) % 2]
            P.copy('act', os_[:, :, (i % 4) * 128:(i % 4 + 1) * 128], pso[:, 0:256].rearrange("p (j t) -> p j t", j=2))
            if i % 4 == 3:
                t0 = (i // 4) * 512
                for j in range(2):
                    P.dma('sp', K.MIXT[j * 128:(j + 1) * 128, t0:t0 + 512], os_[:, j, :], writes=[dkey('MIXT', j * 128, (j + 1) * 128, t0, t0 + 512)])
        P.emit()


RW0 = 652
RW_TS = 256
RW_NSETS = 1


def gen_rwkv(K, l, st):
    nc, P, S = K.nc, K.P, K.S
    TS = RW_TS
    NC = TS // 64
    HB = 512 // TS
    if True:
        sb = lambda n, s, d: st.enter_context(nc.sbuf_tensor(uname(n), list(s), d))
        sets = []
        for bi in range(RW_NSETS):
            farena = sb("r_far%d" % bi, [64, 12, 6, TS], F32)
            barena = sb("r_bar%d" % bi, [64, 16, 6, TS], BF16)
            f2d = farena[:].rearrange("p s h t -> p (s h t)")
            sets.append(dict(
                F=[farena[:, k] for k in range(12)], B=[barena[:, k] for k in range(16)],
                X=f2d[:, 0:20 * (TS + 1)].rearrange("p (i t) -> p i t", i=20),
                Dl=f2d[:, 4 * 6 * TS:4 * 6 * TS + 20 * TS].rearrange("p (i t) -> p i t", i=20),
                XG=sb("r_XG%d" % bi, [128, TS + 1], F32), sgx=sb("r_sgx%d" % bi, [128, TS], F32),
                sgxb=sb("r_sgxb%d" % bi, [128, TS], BF16),
                Zs=sb("r_Zs%d" % bi, [64, 6, 64], BF16), Us=sb("r_Us%d" % bi, [64, 6, 64], BF16),
                WL=sb("r_WL%d" % bi, [64, 6, NC], F32)))
        mua = sb("r_mua", [64, 20], F32)
        mug = sb("r_mug", [128, 1], F32)
        vec = sb("r_vec", [64, 7, 6], F32)
        omka = sb("r_omka", [64, 6], F32)
        wst = sb("r_wst", [128, 384], F32)
        w2 = sb("r_w2", [64, 384], BF16)
        a2 = sb("r_a2", [64, 384], BF16)
        g2 = sb("r_g2", [128, 384], BF16)
        onesb = sb("r_onesb", [64, 64], BF16)
        rm = sb("r_rm", [64, TS], F32)
        mskf = sb("r_mskf", [64, 3, 8, 64], F32)
        msk = sb("r_msk", [64, 3, HB, NC, 64], F32)
        itl = sb("r_itl", [64, HB, NC, 64], F32)
        ST = sb("r_ST", [64, 6, 64], F32)
        STb = sb("r_STb", [64, 6, 64], BF16)
        P.dma('sp', mua[:], K.rw_mua[l], reads=[])
        P.dma('sp', mug[:], K.rw_mug[l], reads=[])
        P.dma('sp', vec[:], K.rw_vec[l], reads=[])
        for (dst, src, rows) in ((w2, K.rw_w2[l], 64), (a2, K.rw_a2[l], 64), (g2, K.rw_g2[l], 128)):
            P.dma('sp', wst[0:rows, :], src, reads=[])
            P.copy('dve', dst[:], wst[0:rows, :])
        P.dma('sp', mskf[:].rearrange("p m c t -> p (m c t)"), K.c_rmsk, reads=[])
        for m in range(3):
            for hb in range(HB):
                P.copy('pool', msk[:, m, hb], mskf[:, m, 0:NC, :])
        for hb in range(HB):
            P.copy('pool', itl[:, hb].rearrange("p c t -> p (c t)"), K.c_itile_sb[:, 0:NC * 64])
        P.ts('dve', omka[:], vec[:, 3, :], -1.0, 1.0, ALU.mult, ALU.add)
        P.memset('dve', rm[:], 1.0)
        P.memset('dve', rm[:, 0:TS:64], 0.0)
        P.memset('dve', ST[:], 0.0)
        P.memset('dve', STb[:], 0.0)
        P.memset('pool', onesb[:], 1.0)
        W0, A0, KK, KA, LNW, LNB, RK = range(7)
        i64 = K.identb[0:64, 0:64]

        def hgroups():
            for h0 in range(0, 6, HB):
                yield h0, min(HB, 6 - h0)

        def mm_blocks(dst_fn, lt, rt, evac):
            for h0, nh in hgroups():
                ps = next_ps(K)
                first = True
                for j in range(nh):
                    for c in range(NC):
                        cc = slice(c * 64, (c + 1) * 64)
                        o = (j * NC + c) * 64
                        P.mm(ps[0:64, o:o + 64], lt[:, h0 + j, cc], rt[:, h0 + j, cc], start=first)
                        first = False
                evac(h0, nh, ps[0:64, 0:nh * TS])

        def superchunk(sc, bs):
            F, B, X, Dl, XG, sgx, sgxb, Zs, Us, WL = (bs[k] for k in ("F", "B", "X", "Dl", "XG", "sgx", "sgxb", "Zs", "Us", "WL"))
            t0 = sc * TS
            for idx in range(20):
                r0 = RW0 + idx * 64
                if sc == 0:
                    P.memset('pool', X[:, idx, 0:1], 0.0)
                    P.dma(('sp', 'pool')[idx % 2], X[:, idx, 1:TS + 1], K.PT[r0:r0 + 64, 0:TS], reads=[dkey('PT', r0, r0 + 64, 0, TS)])
                else:
                    P.dma(('sp', 'pool')[idx % 2], X[:, idx, :], K.PT[r0:r0 + 64, t0 - 1:t0 + TS], reads=[dkey('PT', r0, r0 + 64, t0 - 1, t0 + TS)])
            r0 = RW0 + 1280
            if sc == 0:
                P.memset('pool', XG[:, 0:1], 0.0)
                P.dma('sp', XG[:, 1:TS + 1], K.PT[r0:r0 + 128, 0:TS], reads=[dkey('PT', r0, r0 + 128, 0, TS)])
            else:
                P.dma('sp', XG[:, :], K.PT[r0:r0 + 128, t0 - 1:t0 + TS], reads=[dkey('PT', r0, r0 + 128, t0 - 1, t0 + TS)])
            P.tt('dve', Dl, X[:, :, 0:TS], X[:, :, 1:TS + 1], ALU.subtract)
            P.tt('dve', Dl, Dl, bl(mua[:], TS), ALU.mult)
            P.tt('pool', Dl, Dl, X[:, :, 1:TS + 1], ALU.add)
            P.tt('dve', sgx[:], XG[:, 0:TS], XG[:, 1:TS + 1], ALU.subtract)
            P.stt('dve', sgx[:], sgx[:], mug[:, 0:1], XG[:, 1:TS + 1], ALU.mult, ALU.add)
            P.act(sgxb[:], sgx[:], AF.Sigmoid)
            yield
            rp, kp, vp = F[4], F[5], F[6]
            xw, xa = F[7][:, 0, :], F[7][:, 1, :]
            twb, xab = B[0][:, 0, :], B[0][:, 1, :]
            P.act(twb, xw, AF.Tanh)
            P.copy('act', xab, xa)
            ld, aa, gg, kk, kmod = F[0], F[1], F[8], F[2], F[3]
            for (wt, rhs, dst, func, bi) in ((w2, twb, ld, AF.Sigmoid, W0), (a2, xab, aa, AF.Sigmoid, A0), (g2, sgxb[:], gg, None, None)):
                for h0, nh in hgroups():
                    ps = next_ps(K)
                    for j in range(nh):
                        h = h0 + j
                        P.mm(ps[0:64, j * TS:(j + 1) * TS], wt[:, h * 64:(h + 1) * 64], rhs, start=(j == 0))
                    for j in range(nh):
                        h = h0 + j
                        if func is None:
                            P.copy('act', dst[:, h, :], ps[0:64, j * TS:(j + 1) * TS])
                        else:
                            P.act(dst[:, h, :], ps[0:64, j * TS:(j + 1) * TS], func, bias=vec[:, bi, h:h + 1])
            yield
            P.ts('dve', ld[:], ld[:], -0.6065306597126334, None, ALU.mult)
            kk2b = B[1]
            P.tt('dve', kk[:], kp[:], bl(vec[:, KK, :], TS), ALU.mult)
            P.tt('pool', kk2b[:], kk[:], kk[:], ALU.mult)
            for h0, nh in hgroups():
                ps = next_ps(K)
                for j in range(nh):
                    P.mm(ps[0:64, j * TS:(j + 1) * TS], onesb[:], kk2b[:, h0 + j, :], start=(j == 0))
                P.act(F[9][:, h0:h0 + nh, :], ps[0:64, 0:nh * TS].rearrange("p (h t) -> p h t", h=nh), AF.Sqrt)
            P.ts('dve', F[9][:], F[9][:], 1e-12, None, ALU.max)
            P.recip(F[9][:], F[9][:])
            P.tt('dve', kk[:], kk[:], F[9][:], ALU.mult)
            yield
            P.tt('dve', kmod[:], aa[:], bl(vec[:, KA, :], TS), ALU.mult)
            P.tt('dve', kmod[:], kmod[:], bl(omka[:], TS), ALU.add)
            P.tt('pool', kmod[:], kmod[:], kp[:], ALU.mult)
            yield
            cl, eW, eWm = F[9], F[10], F[11]
            for h in range(6):
                P.op('dve', lambda e, h=h: e.tensor_tensor_scan(cl[:, h, :], rm[:], ld[:, h, :], 0.0, ALU.mult, ALU.add),
                     [rm[:], ld[:, h, :]], [cl[:, h, :]])
            P.act(eW[:], cl[:], AF.Exp)
            P.tt('dve', eWm[:], cl[:], ld[:], ALU.subtract)
            P.act(eWm[:], eWm[:], AF.Exp)
            P.copy('act', WL[:], eW[:, :, 63:TS:64])
            yield
            At, Rt, Bt, Kt = B[2], B[3], B[4], B[5]
            P.stt('dve', At[:], eWm[:], -1.0, kk[:], ALU.mult, ALU.mult)
            P.tt('pool', Rt[:], rp[:], eW[:], ALU.mult)
            P.act(cl[:], cl[:], AF.Exp, scale=-1.0)
            P.tt('dve', eWm[:], kk[:], aa[:], ALU.mult)
            P.tt('dve', Bt[:], eWm[:], cl[:], ALU.mult)
            P.tt('pool', Kt[:], kmod[:], cl[:], ALU.mult)
            yield
            BV = F[0]
            rkrb = B[1]
            P.tt('dve', eWm[:], rp[:], kmod[:], ALU.mult)
            P.tt('dve', rkrb[:], eWm[:], bl(vec[:, RK, :], TS), ALU.mult)
            for h0, nh in hgroups():
                ps = next_ps(K)
                for j in range(nh):
                    P.mm(ps[0:64, j * TS:(j + 1) * TS], onesb[:], rkrb[:, h0 + j, :], start=(j == 0))
                P.tt('dve', BV[:, h0:h0 + nh, :], ps[0:64, 0:nh * TS].rearrange("p (h t) -> p h t", h=nh), vp[:, h0:h0 + nh, :], ALU.mult)
            vpb = B[0]
            P.copy('act', vpb[:], vp[:])
            yield
            Vtok, Btok, Ktok = B[6], B[7], B[8]
            ei = 0
            for (src, dst) in ((vpb, Vtok), (Bt, Btok), (Kt, Ktok)):
                for h0 in range(0, 6, 3):
                    pt = next_psT(K)
                    for j in range(3):
                        for c in range(NC):
                            o = (j * NC + c) * 64
                            P.tr(pt[0:64, o:o + 64], src[:, h0 + j, c * 64:(c + 1) * 64], i64)
                    P.copy('act', dst[:, h0:h0 + 3, :], pt[0:64, 0:3 * TS].rearrange("p (h t) -> p h t", h=3))
                    ei += 1
            yield
            NTm, Nm, Arb, Aak, Ark = B[9], B[10], B[11], B[12], B[13]
            for (dst, lt, rt, mi) in ((NTm, Bt, At, 0), (Nm, At, Bt, 2), (Arb, Bt, Rt, 1), (Aak, Kt, At, 0), (Ark, Kt, Rt, 1)):
                def ev(h0, nh, pv, dst=dst, mi=mi):
                    P.tt('dve', dst[:, h0:h0 + nh, :], pv.rearrange("p (h t) -> p h t", h=nh),
                         msk[:, mi, 0:nh].rearrange("p h c t -> p h (c t)"), ALU.mult)
                mm_blocks(None, lt, rt, ev)
                yield
            yield
            Tt32, Ttb = F[1], B[14]
            for h0, nh in hgroups():
                P.tt('dve', Tt32[:, h0:h0 + nh, :], NTm[:, h0:h0 + nh, :], itl[:, 0:nh].rearrange("p h c t -> p h (c t)"), ALU.add)
            P.copy('act', Ttb[:], Tt32[:])
            cN, cNT, nN, nNT = Nm, NTm, B[15], B[1]
            for lvl in range(5):
                last = (lvl == 4)
                def evN(h0, nh, pv, d=nN):
                    P.copy('act', d[:, h0:h0 + nh, :], pv.rearrange("p (h t) -> p h t", h=nh))
                mm_blocks(None, cNT, cN, evN)
                if not last:
                    def evNT(h0, nh, pv, d=nNT):
                        P.copy('act', d[:, h0:h0 + nh, :], pv.rearrange("p (h t) -> p h t", h=nh))
                    mm_blocks(None, cN, cNT, evNT)
                def evT(h0, nh, pv):
                    P.tt('dve', Tt32[:, h0:h0 + nh, :], Tt32[:, h0:h0 + nh, :], pv.rearrange("p (h t) -> p h t", h=nh), ALU.add)
                    P.copy('act', Ttb[:, h0:h0 + nh, :], Tt32[:, h0:h0 + nh, :])
                mm_blocks(None, nN, Ttb, evT)
                cN, cNT, nN, nNT = nN, nNT, cN, cNT
                yield
            yield
            Yt = F[2]
            for c in range(NC):
                cc = slice(c * 64, (c + 1) * 64)
                pz = next_ps(K)
                for h in range(6):
                    o = slice(h * 64, (h + 1) * 64)
                    P.mm(pz[0:64, o], At[:, h, cc], STb[:, h, :], start=(h == 0))
                    P.mm(pz[0:64, o], Aak[:, h, cc], Vtok[:, h, cc], start=False)
                P.copy('act', Zs[:].rearrange("p h v -> p (h v)"), pz[0:64, 0:384])
                pu_ = next_ps(K)
                for h in range(6):
                    o = slice(h * 64, (h + 1) * 64)
                    P.mm(pu_[0:64, o], Ttb[:, h, cc], Zs[:, h, :], start=(h == 0))
                P.copy('act', Us[:].rearrange("p h v -> p (h v)"), pu_[0:64, 0:384])
                py = next_ps(K)
                for h in range(6):
                    o = slice(h * 64, (h + 1) * 64)
                    P.mm(py[0:64, o], STb[:, h, :], Rt[:, h, cc], start=(h == 0))
                    P.mm(py[0:64, o], Us[:, h, :], Arb[:, h, cc], start=False)
                    P.mm(py[0:64, o], Vtok[:, h, cc], Ark[:, h, cc], start=False)
                P.copy('act', Yt[:, :, cc], py[0:64, 0:384].rearrange("p (h t) -> p h t", h=6))
                pn = next_ps(K)
                for h in range(6):
                    o = slice(h * 64, (h + 1) * 64)
                    P.mm(pn[0:64, o], Btok[:, h, cc], Us[:, h, :], start=(h == 0))
                    P.mm(pn[0:64, o], Ktok[:, h, cc], Vtok[:, h, cc], start=False)
                P.tt('dve', ST[:], ST[:], pn[0:64, 0:384].rearrange("p (h v) -> p h v", h=6), ALU.add)
                P.tt('dve', ST[:], ST[:], bl(WL[:, :, c], 64), ALU.mult)
                P.copy('act', STb[:], ST[:])
                yield
            yield
            yc, sq = F[3], F[5]
            Ytb, sqb = B[4], B[5]
            P.copy('act', Ytb[:], Yt[:])
            for h0, nh in hgroups():
                ps = next_ps(K)
                for j in range(nh):
                    P.mm(ps[0:64, j * TS:(j + 1) * TS], onesb[:], Ytb[:, h0 + j, :], start=(j == 0))
                P.stt('dve', yc[:, h0:h0 + nh, :], ps[0:64, 0:nh * TS].rearrange("p (h t) -> p h t", h=nh), -1.0 / 64,
                      Yt[:, h0:h0 + nh, :], ALU.mult, ALU.add)
            P.tt('pool', sqb[:], yc[:], yc[:], ALU.mult)
            for h0, nh in hgroups():
                ps = next_ps(K)
                for j in range(nh):
                    P.mm(ps[0:64, j * TS:(j + 1) * TS], onesb[:], sqb[:, h0 + j, :], start=(j == 0))
                P.act(sq[:, h0:h0 + nh, :], ps[0:64, 0:nh * TS].rearrange("p (h t) -> p h t", h=nh), AF.Sqrt, bias=64e-5, scale=1.0 / 64)
            P.recip(sq[:], sq[:])
            P.tt('dve', yc[:], yc[:], sq[:], ALU.mult)
            P.tt('dve', yc[:], yc[:], bl(vec[:, LNW, :], TS), ALU.mult)
            P.tt('pool', yc[:], yc[:], bl(vec[:, LNB, :], TS), ALU.add)
            P.tt('pool', yc[:], yc[:], BV[:], ALU.add)
            P.tt('dve', yc[:], yc[:], gg[:], ALU.mult)
            for h in range(6):
                r0 = 256 + h * 64
                P.dma(('sp', 'pool')[h % 2], K.MIXT[r0:r0 + 64, t0:t0 + TS], yc[:, h, :], writes=[dkey('MIXT', r0, r0 + 64, t0, t0 + TS)])

        NSC = S // TS
        LAG = 14
        active = []
        nxt = 0
        while nxt < NSC or active:
            if nxt < NSC and len(active) < RW_NSETS and (not active or active[-1][1] >= LAG):
                active.append([superchunk(nxt, sets[nxt % RW_NSETS]), 0])
                nxt += 1
            for a in list(active):
                try:
                    next(a[0])
                    a[1] += 1
                except StopIteration:
                    active.remove(a)
            yield


def phase_rwkv(K, l):
    with ExitStack() as st:
        for _ in gen_rwkv(K, l, st):
            pass
        K.P.emit()


RW2_NSETS = 2
RW2_LAG = 12


def gen_rwkv2(K, l, st):
    nc, P, S = K.nc, K.P, K.S
    TS = 256
    NC = TS // 64
    sb = lambda n, s, d: st.enter_context(nc.sbuf_tensor(uname(n), list(s), d))
    sets = []
    for bi in range(RW2_NSETS):
        farena = sb("q_far%d" % bi, [128, 12, 3, TS], F32)
        barena = sb("q_bar%d" % bi, [128, 16, 3, TS], BF16)
        f2d = farena[:].rearrange("p s h t -> p (s h t)")
        sets.append(dict(
            F=[farena[:, k] for k in range(12)], B=[barena[:, k] for k in range(16)],
            X=f2d[:, 0:10 * (TS + 1)].rearrange("p (i t) -> p i t", i=10),
            Dl=f2d[:, 4 * 3 * TS:4 * 3 * TS + 10 * TS].rearrange("p (i t) -> p i t", i=10),
            XG=sb("q_XG%d" % bi, [128, TS + 1], F32), sgx=sb("q_sgx%d" % bi, [128, TS], F32),
            sgxb=sb("q_sgxb%d" % bi, [128, TS], BF16), Zs=sb("q_Zs%d" % bi, [128, 3, 64], BF16),
            Us=sb("q_Us%d" % bi, [128, 3, 64], BF16), WL=sb("q_WL%d" % bi, [128, 3, NC], F32)))
    mua = sb("q_mua", [128, 10], F32)
    mug = sb("q_mug", [128, 1], F32)
    vec = sb("q_vec", [128, 7, 3], F32)
    omka = sb("q_omka", [128, 3], F32)
    wst = sb("q_wst", [128, 384], F32)
    wa2 = sb("q_wa2", [128, 384], BF16)
    g2 = sb("q_g2", [128, 384], BF16)
    bd = sb("q_bd", [128, 128], BF16)
    rm = sb("q_rm", [128, TS], F32)
    msk = sb("q_msk", [128, 3, 2, NC, 64], F32)
    itl = sb("q_itl", [128, 2, NC, 64], F32)
    ST = sb("q_ST", [128, 3, 64], F32)
    STb = sb("q_STb", [128, 3, 64], BF16)
    P.dma('sp', mua[:], K.rw2_mua[l], reads=[])
    P.dma('sp', mug[:], K.rw_mug[l], reads=[])
    P.dma('sp', vec[:], K.rw2_vec[l], reads=[])
    P.dma('sp', wst[0:64, :], K.rw_w2[l], reads=[])
    P.dma('sp', wst[64:128, :], K.rw_a2[l], reads=[])
    P.copy('dve', wa2[:], wst[:])
    P.dma('sp', wst[:], K.rw_g2[l], reads=[])
    P.copy('dve', g2[:], wst[:])
    rmv = K.c_rmsk.rearrange("p (m c t) -> p m c t", m=3, c=8)
    for g in range(2):
        for m in range(3):
            for j in range(2):
                P.dma('sp', msk[64 * g:64 * g + 64, m, j], rmv[:, m, 0:NC, :], reads=[])
    for g in range(2):
        for j in range(2):
            P.dma('sp', itl[64 * g:64 * g + 64, j].rearrange("p c t -> p (c t)"), K.c_itile[:, 0:NC * 64], reads=[])
    P.ts('dve', omka[:], vec[:, 3, :], -1.0, 1.0, ALU.mult, ALU.add)
    P.memset('dve', rm[:], 1.0)
    P.memset('dve', rm[:, 0:TS:64], 0.0)
    P.memset('dve', ST[:], 0.0)
    P.memset('dve', STb[:], 0.0)
    P.memset('pool', bd[:], 0.0)
    P.memset('pool', bd[0:64, 0:64], 1.0)
    P.memset('pool', bd[64:128, 64:128], 1.0)
    W0, A0, KK, KA, LNW, LNB, RK = range(7)
    GR = ((0, 2), (2, 1))

    def hp(g):
        return slice(64 * g, 64 * g + 64)

    def mm_blocks(lt, rt, evac):
        for hh0, n in GR:
            ps = next_ps(K)
            for g in range(2):
                first = True
                for j in range(n):
                    for c in range(NC):
                        cc = slice(c * 64, (c + 1) * 64)
                        o = (j * NC + c) * 64
                        P.mm(ps[hp(g), o:o + 64], lt[hp(g), hh0 + j, cc], rt[hp(g), hh0 + j, cc], start=first)
                        first = False
            evac(hh0, n, ps[:, 0:n * TS].rearrange("p (h t) -> p h t", h=n))

    def colsum(dst_fn, src):
        for hh0, n in GR:
            ps = next_ps(K)
            for j in range(n):
                P.mm(ps[:, j * TS:(j + 1) * TS], bd[:], src[:, hh0 + j, :], start=(j == 0))
            dst_fn(hh0, n, ps[:, 0:n * TS].rearrange("p (h t) -> p h t", h=n))

    def superchunk(sc, bs):
        F, B, X, Dl, XG, sgx, sgxb, Zs, Us, WL = (bs[k] for k in ("F", "B", "X", "Dl", "XG", "sgx", "sgxb", "Zs", "Us", "WL"))
        t0 = sc * TS
        di = 0
        for q in range(3):
            for h in range(6):
                g, hh = divmod(h, 3)
                r0 = RW0 + q * 384 + h * 64
                dst = X[hp(g), q * 3 + hh, :]
                eng = ('sp', 'pool')[di % 2]
                di += 1
                if sc == 0:
                    P.memset('pool', dst[:, 0:1], 0.0)
                    P.dma(eng, dst[:, 1:TS + 1], K.PT[r0:r0 + 64, 0:TS], reads=[dkey('PT', r0, r0 + 64, 0, TS)])
                else:
                    P.dma(eng, dst, K.PT[r0:r0 + 64, t0 - 1:t0 + TS], reads=[dkey('PT', r0, r0 + 64, t0 - 1, t0 + TS)])
        for g in range(2):
            r0 = RW0 + 1152 + 64 * g
            dst = X[hp(g), 9, :]
            if sc == 0:
                P.memset('pool', dst[:, 0:1], 0.0)
                P.dma('sp', dst[:, 1:TS + 1], K.PT[r0:r0 + 64, 0:TS], reads=[dkey('PT', r0, r0 + 64, 0, TS)])
            else:
                P.dma('sp', dst, K.PT[r0:r0 + 64, t0 - 1:t0 + TS], reads=[dkey('PT', r0, r0 + 64, t0 - 1, t0 + TS)])
        r0 = RW0 + 1280
        if sc == 0:
            P.memset('pool', XG[:, 0:1], 0.0)
            P.dma('sp', XG[:, 1:TS + 1], K.PT[r0:r0 + 128, 0:TS], reads=[dkey('PT', r0, r0 + 128, 0, TS)])
        else:
            P.dma('sp', XG[:, :], K.PT[r0:r0 + 128, t0 - 1:t0 + TS], reads=[dkey('PT', r0, r0 + 128, t0 - 1, t0 + TS)])
        P.tt('dve', Dl, X[:, :, 0:TS], X[:, :, 1:TS + 1], ALU.subtract)
        P.tt('dve', Dl, Dl, bl(mua[:], TS), ALU.mult)
        P.tt('pool', Dl, Dl, X[:, :, 1:TS + 1], ALU.add)
        P.tt('dve', sgx[:], XG[:, 0:TS], XG[:, 1:TS + 1], ALU.subtract)
        P.stt('dve', sgx[:], sgx[:], mug[:, 0:1], XG[:, 1:TS + 1], ALU.mult, ALU.add)
        P.act(sgxb[:], sgx[:], AF.Sigmoid)
        yield
        rp, kp, vp = F[4], F[5], F[6]
        txb = B[0][:, 0, :]
        P.act(txb[0:64], F[7][0:64, 0, :], AF.Tanh)
        P.copy('act', txb[64:128], F[7][64:128, 0, :])
        ld, aa, gg, kk, kmod = F[0], F[1], F[8], F[2], F[3]
        for (kind, dst, bi) in (('w', ld, W0), ('a', aa, A0), ('g', gg, None)):
            for hh0, n in GR:
                ps = next_ps(K)
                for g in range(2):
                    for j in range(n):
                        h = 3 * g + hh0 + j
                        hs = slice(h * 64, (h + 1) * 64)
                        o = ps[hp(g), j * TS:(j + 1) * TS]
                        if kind == 'w':
                            P.mm(o, wa2[0:64, hs], txb[0:64], start=(j == 0))
                        elif kind == 'a':
                            P.mm(o, wa2[64:128, hs], txb[64:128], start=(j == 0))
                        else:
                            P.mm(o, g2[:, hs], sgxb[:], start=(j == 0))
                for j in range(n):
                    hh = hh0 + j
                    if bi is None:
                        P.copy('act', dst[:, hh, :], ps[:, j * TS:(j + 1) * TS])
                    else:
                        P.act(dst[:, hh, :], ps[:, j * TS:(j + 1) * TS], AF.Sigmoid, bias=vec[:, bi, hh:hh + 1])
        yield
        P.ts('dve', ld[:], ld[:], -0.6065306597126334, None, ALU.mult)
        kk2b = B[1]
        P.tt('dve', kk[:], kp[:], bl(vec[:, KK, :], TS), ALU.mult)
        P.tt('pool', kk2b[:], kk[:], kk[:], ALU.mult)
        colsum(lambda hh0, n, pv: P.act(F[9][:, hh0:hh0 + n, :], pv, AF.Sqrt), kk2b)
        P.ts('dve', F[9][:], F[9][:], 1e-12, None, ALU.max)
        P.recip(F[9][:], F[9][:])
        P.tt('dve', kk[:], kk[:], F[9][:], ALU.mult)
        P.tt('dve', kmod[:], aa[:], bl(vec[:, KA, :], TS), ALU.mult)
        P.tt('dve', kmod[:], kmod[:], bl(omka[:], TS), ALU.add)
        P.tt('pool', kmod[:], kmod[:], kp[:], ALU.mult)
        yield
        cl, eW, eWm = F[9], F[10], F[11]
        for hh in range(3):
            P.op('dve', lambda e, hh=hh: e.tensor_tensor_scan(cl[:, hh, :], rm[:], ld[:, hh, :], 0.0, ALU.mult, ALU.add),
                 [rm[:], ld[:, hh, :]], [cl[:, hh, :]])
        P.act(eW[:], cl[:], AF.Exp)
        P.tt('dve', eWm[:], cl[:], ld[:], ALU.subtract)
        P.act(eWm[:], eWm[:], AF.Exp)
        P.copy('act', WL[:], eW[:, :, 63:TS:64])
        yield
        At, Rt, Bt, Kt = B[2], B[3], B[4], B[5]
        P.stt('dve', At[:], eWm[:], -1.0, kk[:], ALU.mult, ALU.mult)
        P.tt('pool', Rt[:], rp[:], eW[:], ALU.mult)
        P.act(cl[:], cl[:], AF.Exp, scale=-1.0)
        P.tt('dve', eWm[:], kk[:], aa[:], ALU.mult)
        P.tt('dve', Bt[:], eWm[:], cl[:], ALU.mult)
        P.tt('pool', Kt[:], kmod[:], cl[:], ALU.mult)
        yield
        BV = F[0]
        rkrb = B[1]
        P.tt('dve', eWm[:], rp[:], kmod[:], ALU.mult)
        P.tt('dve', rkrb[:], eWm[:], bl(vec[:, RK, :], TS), ALU.mult)
        colsum(lambda hh0, n, pv: P.tt('dve', BV[:, hh0:hh0 + n, :], pv, vp[:, hh0:hh0 + n, :], ALU.mult), rkrb)
        vpb = B[0]
        P.copy('act', vpb[:], vp[:])
        yield
        Vtok, Btok, Ktok = B[6], B[7], B[8]
        for (src, dst) in ((vpb, Vtok), (Bt, Btok), (Kt, Ktok)):
            pt = next_psT(K)
            for g in range(2):
                for hh in range(3):
                    for c in range(NC):
                        o = (hh * NC + c) * 64
                        P.tr(pt[hp(g), o:o + 64], src[hp(g), hh, c * 64:(c + 1) * 64], K.identb[hp(g), hp(g)])
            P.copy('act', dst[:], pt[:, 0:3 * TS].rearrange("p (h t) -> p h t", h=3))
        yield
        NTm, Nm, Arb, Aak, Ark = B[9], B[10], B[11], B[12], B[13]
        for (dst, lt, rt, mi) in ((NTm, Bt, At, 0), (Nm, At, Bt, 2), (Arb, Bt, Rt, 1), (Aak, Kt, At, 0), (Ark, Kt, Rt, 1)):
            def ev(hh0, n, pv, dst=dst, mi=mi):
                P.tt('dve', dst[:, hh0:hh0 + n, :], pv, msk[:, mi, 0:n].rearrange("p h c t -> p h (c t)"), ALU.mult)
            mm_blocks(lt, rt, ev)
            yield
        yield
        Tt32, Ttb = F[1], B[14]
        for hh0, n in GR:
            P.tt('dve', Tt32[:, hh0:hh0 + n, :], NTm[:, hh0:hh0 + n, :], itl[:, 0:n].rearrange("p h c t -> p h (c t)"), ALU.add)
        P.copy('act', Ttb[:], Tt32[:])
        cN, cNT, nN, nNT = Nm, NTm, B[15], B[1]
        for lvl in range(5):
            last = (lvl == 4)
            def evN(hh0, n, pv, d=nN):
                P.copy('act', d[:, hh0:hh0 + n, :], pv)
            mm_blocks(cNT, cN, evN)
            if not last:
                def evNT(hh0, n, pv, d=nNT):
                    P.copy('act', d[:, hh0:hh0 + n, :], pv)
                mm_blocks(cN, cNT, evNT)
            def evT(hh0, n, pv):
                P.tt('dve', Tt32[:, hh0:hh0 + n, :], Tt32[:, hh0:hh0 + n, :], pv, ALU.add)
                P.copy('act', Ttb[:, hh0:hh0 + n, :], Tt32[:, hh0:hh0 + n, :])
            mm_blocks(nN, Ttb, evT)
            cN, cNT, nN, nNT = nN, nNT, cN, cNT
            yield
        yield
        Yt = F[2]
        for c in range(NC):
            cc = slice(c * 64, (c + 1) * 64)
            pz = next_ps(K)
            for g in range(2):
                for hh in range(3):
                    o = slice(hh * 64, (hh + 1) * 64)
                    P.mm(pz[hp(g), o], At[hp(g), hh, cc], STb[hp(g), hh, :], start=(hh == 0))
                    P.mm(pz[hp(g), o], Aak[hp(g), hh, cc], Vtok[hp(g), hh, cc], start=False)
            P.copy('act', Zs[:].rearrange("p h v -> p (h v)"), pz[:, 0:192])
            pu_ = next_ps(K)
            for g in range(2):
                for hh in range(3):
                    o = slice(hh * 64, (hh + 1) * 64)
                    P.mm(pu_[hp(g), o], Ttb[hp(g), hh, cc], Zs[hp(g), hh, :], start=(hh == 0))
            P.copy('act', Us[:].rearrange("p h v -> p (h v)"), pu_[:, 0:192])
            py = next_ps(K)
            for g in range(2):
                for hh in range(3):
                    o = slice(hh * 64, (hh + 1) * 64)
                    P.mm(py[hp(g), o], STb[hp(g), hh, :], Rt[hp(g), hh, cc], start=(hh == 0))
                    P.mm(py[hp(g), o], Us[hp(g), hh, :], Arb[hp(g), hh, cc], start=False)
                    P.mm(py[hp(g), o], Vtok[hp(g), hh, cc], Ark[hp(g), hh, cc], start=False)
            P.copy('act', Yt[:, :, cc], py[:, 0:192].rearrange("p (h t) -> p h t", h=3))
            pn = next_ps(K)
            for g in range(2):
                for hh in range(3):
                    o = slice(hh * 64, (hh + 1) * 64)
                    P.mm(pn[hp(g), o], Btok[hp(g), hh, cc], Us[hp(g), hh, :], start=(hh == 0))
                    P.mm(pn[hp(g), o], Ktok[hp(g), hh, cc], Vtok[hp(g), hh, cc], start=False)
            P.tt('dve', ST[:], ST[:], pn[:, 0:192].rearrange("p (h v) -> p h v", h=3), ALU.add)
            P.tt('dve', ST[:], ST[:], bl(WL[:, :, c], 64), ALU.mult)
            P.copy('act', STb[:], ST[:])
            yield
        yield
        yc, sq = F[3], F[5]
        Ytb, sqb = B[4], B[5]
        P.copy('act', Ytb[:], Yt[:])
        colsum(lambda hh0, n, pv: P.stt('dve', yc[:, hh0:hh0 + n, :], pv, -1.0 / 64, Yt[:, hh0:hh0 + n, :], ALU.mult, ALU.add), Ytb)
        P.tt('pool', sqb[:], yc[:], yc[:], ALU.mult)
        colsum(lambda hh0, n, pv: P.act(sq[:, hh0:hh0 + n, :], pv, AF.Sqrt, bias=64e-5, scale=1.0 / 64), sqb)
        P.recip(sq[:], sq[:])
        P.tt('dve', yc[:], yc[:], sq[:], ALU.mult)
        P.tt('dve', yc[:], yc[:], bl(vec[:, LNW, :], TS), ALU.mult)
        P.tt('pool', yc[:], yc[:], bl(vec[:, LNB, :], TS), ALU.add)
        P.tt('pool', yc[:], yc[:], BV[:], ALU.add)
        P.tt('dve', yc[:], yc[:], gg[:], ALU.mult)
        for h in range(6):
            g, hh = divmod(h, 3)
            r0 = 256 + h * 64
            P.dma(('sp', 'pool')[h % 2], K.MIXT[r0:r0 + 64, t0:t0 + TS], yc[hp(g), hh, :], writes=[dkey('MIXT', r0, r0 + 64, t0, t0 + TS)])

    NSC = S // TS
    active = []
    nxt = 0
    done_steps = 0
    TOT = NSC * 31.0
    while nxt < NSC or active:
        if nxt < NSC and len(active) < RW2_NSETS and (not active or active[-1][1] >= RW2_LAG):
            active.append([superchunk(nxt, sets[nxt % RW2_NSETS]), 0])
            nxt += 1
        for a in list(active):
            try:
                next(a[0])
                a[1] += 1
                done_steps += 1
            except StopIteration:
                active.remove(a)
        yield min(0.999, done_steps / TOT)


def phase_rwkv2(K, l):
    with ExitStack() as st:
        for _ in gen_rwkv2(K, l, st):
            pass
        K.P.emit()
```

```python
import numpy as np
from contextlib import ExitStack
import concourse.bass as bass
import concourse.mybir as mybir
from concourse.bass_utils import run_bass_kernel_spmd

F32 = mybir.dt.float32
BF16 = mybir.dt.bfloat16
AF = mybir.ActivationFunctionType
ALU = mybir.AluOpType
AX = mybir.AxisListType

D = 1024
DFF = 2816
DIN = 3608
NEGB = -30000.0
N_DMA_SEMS = 16
N_SDMA_SEMS = 8
SAME_ENGINE_SYNC = True


def _box(ap):
    t = ap.tensor
    pat = ap.ap
    off = int(ap.offset)
    fsz = 1
    for s in t.shape[1:]:
        fsz *= int(s)
    pstep, pcnt = pat[0]
    p0 = off // fsz
    f0 = off % fsz
    p1 = p0 + 1 if pstep == 0 else p0 + (pcnt - 1) * (pstep // fsz) + 1
    ext = 0
    for st, cnt in pat[1:]:
        ext += abs(st) * (cnt - 1)
    if t.name.startswith('ps'):
        return (t.name, 0, 128, 0, 1 << 20)
    return (t.name, p0, p1, f0, f0 + ext + 1)


class Prog:
    def __init__(self, nc, st):
        self.nc = nc
        self.engs = {'pe': nc.tensor, 'act': nc.scalar, 'dve': nc.vector, 'pool': nc.gpsimd, 'sp': nc.sync}
        self.q = {e: [] for e in self.engs}
        self.tick = {e: 0 for e in self.engs}
        self.recs = {}
        self.seen = {e: {} for e in self.engs}
        self.dma_tot = {}
        self.dma_next = {'h': 0, 's': 0}
        self.sem = {}
        for e in self.engs:
            self.sem[e] = st.enter_context(nc.semaphore('s_' + e))
        for k in range(N_DMA_SEMS):
            self.sem['dma%d' % k] = st.enter_context(nc.semaphore('s_dma%d' % k))
            self.dma_tot['dma%d' % k] = 0
        for k in range(N_SDMA_SEMS):
            self.sem['sdma%d' % k] = st.enter_context(nc.semaphore('s_sdma%d' % k))
            self.dma_tot['sdma%d' % k] = 0

    def _deps(self, reads, writes):
        waits = {}
        for (is_w, lst) in ((False, reads), (True, writes)):
            for (key, p0, p1, f0, f1) in lst:
                for r in self.recs.get(key, ()):
                    if r[0] < p1 and p0 < r[1] and r[2] < f1 and f0 < r[3] and (is_w or r[6]):
                        if waits.get(r[4], 0) < r[5]:
                            waits[r[4]] = r[5]
        return waits

    def _record(self, semname, val, reads, writes):
        for (is_w, lst) in ((False, reads), (True, writes)):
            for (key, p0, p1, f0, f1) in lst:
                L = self.recs.get(key, [])
                newL = []
                for r in L:
                    cov = (p0 <= r[0] and r[1] <= p1 and f0 <= r[2] and r[3] <= f1)
                    if cov and (is_w or (not r[6] and r[4] == semname)):
                        continue
                    newL.append(r)
                newL.append([p0, p1, f0, f1, semname, val, is_w])
                self.recs[key] = newL

    @staticmethod
    def _boxes(lst):
        out = []
        for x in lst:
            if x is None or isinstance(x, (int, float)):
                continue
            out.append(x if isinstance(x, tuple) else _box(x))
        return out

    def op(self, eng, fn, reads=(), writes=()):
        reads = self._boxes(reads)
        writes = self._boxes(writes)
        waits = self._deps(reads, writes)
        if not SAME_ENGINE_SYNC or eng == 'pe':
            waits.pop(eng, None)
        self.tick[eng] += 1
        self.q[eng].append((waits, fn, (eng, 1)))
        self._record(eng, self.tick[eng], reads, writes)

    def dma(self, eng, out, in_, reads=None, writes=None, **kw):
        r = self._boxes(reads if reads is not None else [in_])
        w = self._boxes(writes if writes is not None else [out])
        waits = self._deps(r, w)
        if eng == 'pool':
            sname = 'sdma%d' % (self.dma_next['s'] % N_SDMA_SEMS)
            self.dma_next['s'] += 1
        else:
            sname = 'dma%d' % (self.dma_next['h'] % N_DMA_SEMS)
            self.dma_next['h'] += 1
        if self.dma_tot[sname] > 0 and waits.get(sname, 0) < self.dma_tot[sname]:
            waits[sname] = self.dma_tot[sname]
        self.dma_tot[sname] += 16
        self.q[eng].append((waits, (lambda e, o=out, i=in_, kw=kw: e.dma_start(out=o, in_=i, **kw)), (sname, 16)))
        self._record(sname, self.dma_tot[sname], r, w)

    def emit(self):
        nc = self.nc
        final = {e: self.tick[e] for e in self.engs}
        dfinal = dict(self.dma_tot)
        with nc.Block() as block:
            def run(ename, engine):
                seen = self.seen[ename]
                for waits, fn, inc in self.q[ename]:
                    for s, v in waits.items():
                        if seen.get(s, 0) >= v:
                            continue
                        seen[s] = v
                        engine.wait_ge(self.sem[s], v)
                    fn(engine).then_inc(self.sem[inc[0]], inc[1])
                for s, v in final.items():
                    if s != ename and v > seen.get(s, 0):
                        seen[s] = v
                        engine.wait_ge(self.sem[s], v)
                for s, v in dfinal.items():
                    if v > seen.get(s, 0):
                        seen[s] = v
                        engine.wait_ge(self.sem[s], v)
                self.q[ename] = []

            block.sync(lambda e: run('sp', e))
            block.tensor(lambda e: run('pe', e))
            block.scalar(lambda e: run('act', e))
            block.vector(lambda e: run('dve', e))
            block.gpsimd(lambda e: run('pool', e))
        self.recs = {}

    def mm(self, out, lhsT, rhs, start=True, stop=True):
        self.op('pe', lambda e: e.matmul(out, lhsT, rhs, start=start, stop=stop, skip_group_check=True),
                [lhsT, rhs], [out])

    def tr(self, out, in_, ident):
        self.op('pe', lambda e: e.transpose(out, in_, ident), [in_, ident], [out])

    def act(self, out, in_, func, bias=0.0, scale=1.0, accum=None, eng='act'):
        kw = {}
        if accum is not None:
            kw['accum_out'] = accum
        self.op('act', lambda e: e.activation(out, in_, func, bias=bias, scale=scale, **kw),
                [in_, bias, scale], [out, accum])

    def tt(self, eng, out, a, b, op):
        self.op(eng, lambda e: e.tensor_tensor(out, a, b, op), [a, b], [out])

    def ts(self, eng, out, a, s1, s2, op0, op1=None):
        if op1 is None:
            self.op(eng, lambda e: e.tensor_scalar(out, a, s1, None, op0), [a, s1], [out])
        else:
            self.op(eng, lambda e: e.tensor_scalar(out, a, s1, s2, op0, op1), [a, s1, s2], [out])

    def stt(self, eng, out, a, s, b, op0, op1):
        eng = 'dve'
        self.op(eng, lambda e: e.scalar_tensor_tensor(out, a, s, b, op0, op1), [a, s, b], [out])

    def copy(self, eng, out, in_):
        if eng == 'act':
            self.op('act', lambda e: e.copy(out, in_), [in_], [out])
        else:
            self.op(eng, lambda e: e.tensor_copy(out, in_), [in_], [out])

    def memset(self, eng, ap, val):
        self.op(eng, lambda e: e.memset(ap, val), [], [ap])

    def recip(self, out, in_):
        self.op('dve', lambda e: e.reciprocal(out, in_), [in_], [out])


class Ctx:
    pass


_UID = [0]


def uname(n):
    _UID[0] += 1
    return "%s_u%d" % (n, _UID[0])


def dkey(name, r0, r1, c0, c1):
    return (name, r0, r1, c0, c1)


def build_program(S, depth, dbg=False, stub=False, only=('nsa', 'rw', 'ml')):
    NT = S // 128
    NCH = S // 512
    nc = bass.Bass("TRN2", target_bir_lowering=False)
    K = Ctx()
    K.nc, K.S, K.NT, K.NCH, K.depth = nc, S, NT, NCH, depth
    K.stub = stub
    K.only = only

    def din(name, shape, dt=F32):
        return nc.dram_tensor(name, list(shape), dt, kind="ExternalInput").ap()

    L = depth
    K.x = din("x", [S, D])
    K.c = din("c", [128, 8])
    K.w_mod = din("w_mod", [L, D, 6 * D])
    K.b_mod = din("b_mod", [L, 6 * D])
    K.gvec = {n: din(n, [L, D]) for n in ("g_pre_mix", "g_post_mix", "g_pre_ffn", "g_post_ffn")}
    K.w_in = din("w_in", [L, D, DIN])
    K.w_out = din("w_out", [L, D, D])
    K.ffn_g = din("ffn_w_gate", [L, D, DFF])
    K.ffn_u = din("ffn_w_up", [L, D, DFF])
    K.ffn_d = din("ffn_w_down", [L, DFF, D])
    K.ident = din("ident", [128, 128])
    K.tri_in = din("tri_le", [128, 128])
    ncmp = S // 16 - 1
    NN = (ncmp + 127) // 128
    NS = S // 64
    K.c_cmpb = din("c_cmpb", [NN, 128, S])
    K.c_E = din("c_E", [NS, S])
    K.c_caus = din("c_caus", [128, 128])
    K.c_wlow = din("c_wlow", [128, 128])
    K.c_vm = din("c_vm", [S, NS])
    K.c_am = din("c_am", [S, NS])
    K.c_ov = din("c_ov", [NN, 128, NS])
    K.nsa_gb = din("nsa_gate_b", [L, 12])
    K.nsa_og = din("nsa_out_g", [L, 256])
    K.nsa_w1 = [din("nsa_w1k", [L, 64, 32, 64]), din("nsa_w1v", [L, 64, 32, 64])]
    K.nsa_w2 = [din("nsa_ck_w2", [L, 64, 64]), din("nsa_cv_w2", [L, 64, 64])]
    K.nsa_pe = [din("nsa_pekT", [L, 64, 32]), din("nsa_pevT", [L, 64, 32])]
    K.rw_mua = din("rw_mua", [L, 64, 20])
    K.rw_mug = din("rw_mug", [L, 128, 1])
    K.rw_vec = din("rw_vec", [L, 64, 7, 6])
    K.rw2_mua = din("rw2_mua", [L, 128, 10])
    K.rw2_vec = din("rw2_vec", [L, 128, 7, 3])
    K.rw_w2 = din("rw_w2", [L, 64, 384])
    K.rw_a2 = din("rw_a2", [L, 64, 384])
    K.rw_g2 = din("rw_g2", [L, 128, 384])
    K.c_rmsk = din("c_rmsk", [64, 3 * 8 * 64])
    K.c_itile = din("c_itile", [64, 512])
    K.ml_cw = din("ml_cw", [L, 64, 12, 4])
    K.ml_cb = din("ml_cb", [L, 64, 12])
    K.ml_gb = din("ml_gb", [L, 12])
    K.ml_ng = din("ml_norm_g", [L, 384])
    K.out = nc.dram_tensor("out", [S, D], F32, kind="ExternalOutput").ap()
    okind = "ExternalOutput" if dbg else "Internal"
    K.PT = nc.dram_tensor("PT", [DIN, S], F32, kind=okind).ap()
    K.MIXT = nc.dram_tensor("MIXT", [D, S], F32, kind=okind).ap()
    K.X1 = nc.dram_tensor("X1", [S, D], F32, kind="Internal").ap()
    K.X2 = nc.dram_tensor("X2", [S, D], F32, kind="Internal").ap()

    with ExitStack() as gst:
        P = Prog(nc, gst)
        K.P = P
        sbg = lambda n, s, d: gst.enter_context(nc.sbuf_tensor(uname(n), list(s), d))
        K.ps = [gst.enter_context(nc.psum_tensor("psb%d" % i, [128, 512], F32)) for i in range(6)]
        K.psT = [gst.enter_context(nc.psum_tensor("psT%d" % i, [128, 1024], BF16)) for i in range(2)]
        K.psn = 0
        K.psTn = 0
        K.identf = sbg("identf", [128, 128], F32)
        K.identb = sbg("identb", [128, 128], BF16)
        K.ones1 = sbg("ones1", [1, 128], F32)
        K.MODROW = nc.dram_tensor("MODROW", [L, 6 * D], F32, kind="Internal").ap()
        K.trif = sbg("trif", [128, 128], F32)
        K.onesf = sbg("onesf", [128, 128], F32)
        P.dma('sp', K.trif[:], K.tri_in, reads=[])
        P.memset('pool', K.onesf[:], 1.0)
        K.c_itile_sb = sbg("itile_sb", [64, 512], F32)
        P.dma('sp', K.c_itile_sb[:], K.c_itile, reads=[])
        P.dma('sp', K.identf[:], K.ident, reads=[])
        P.copy('dve', K.identb[:], K.identf[:])
        P.memset('dve', K.ones1[:], 1.0)

        phase_mod(K)
        P.emit()
        xin = K.x
        for l in range(depth):
            phase_proj(K, l, xin)
            P.emit()
            phase_mixers(K, l)
            xmid = K.X1
            xout = K.out if l == depth - 1 else K.X2
            phase_out(K, l, xin, xmid, xout)
            P.emit()
            xin = xout
    return nc


def next_ps(K):
    p = K.ps[K.psn % 6]
    K.psn += 1
    return p


def next_psT(K):
    p = K.psT[K.psTn % 2]
    K.psTn += 1
    return p


def phase_mod(K):
    nc, P = K.nc, K.P
    with ExitStack() as st:
        sb = lambda n, s, d: st.enter_context(nc.sbuf_tensor(uname(n), list(s), d))
        ct = sb("m_c", [128, 8], F32)
        cs = sb("m_cs", [128, 8], F32)
        wb = [sb("m_w%d" % i, [128, 8, 512], F32) for i in range(6)]
        bm = sb("m_b", [1, K.depth, 6 * D], F32)
        mrow = [sb("m_row%d" % i, [1, 512], F32) for i in range(2)]
        P.dma('sp', ct[:], K.c, reads=[])
        P.dma('sp', bm[:], K.b_mod.rearrange("(o l) n -> o l n", o=1), reads=[])
        P.act(cs[:], ct[:], AF.Silu)
        i = 0
        for l in range(K.depth):
            for cb in range(12):
                w = wb[i % 6]
                i += 1
                src = K.w_mod[l, :, cb * 512:(cb + 1) * 512].rearrange("(k p) n -> p k n", p=128)
                P.dma(('sp', 'pool', 'act')[i % 3], w[:], src, reads=[])
                ps = next_ps(K)
                for k in range(8):
                    P.mm(ps[0:1, :], cs[:, k:k + 1], w[:, k, :], start=(k == 0), stop=(k == 7))
                mr = mrow[i % 2]
                P.tt('dve', mr[0:1, :], ps[0:1, :], bm[0:1, l, cb * 512:(cb + 1) * 512], ALU.add)
                P.dma('sp', K.MODROW[l:l + 1, cb * 512:(cb + 1) * 512], mr[0:1, :],
                      writes=[dkey('MODROW', l, l + 1, cb * 512, (cb + 1) * 512)])
        P.emit()


def phase_bcast(K, l, st, pieces):
    nc, P = K.nc, K.P
    sb = lambda n, s, d: st.enter_context(nc.sbuf_tensor(uname(n), list(s), d))
    K.bc = {}
    g = sb("b_g", [128, D], F32)
    names = ("g_pre_mix", "g_post_mix", "g_pre_ffn", "g_post_ffn")
    plan = [(0, 1, 'copy', None), (1, 0, 'scale', 0), (2, 2, 'mul', 1),
            (3, 4, 'copy', None), (4, 3, 'scale', 2), (5, 5, 'mul', 3)]
    for piece, dst, kind, gi in plan:
        if dst not in pieces:
            continue
        o = sb("bcast%d" % dst, [128, D], F32)
        K.bc[dst] = o
        P.dma('sp', o[:], K.MODROW[l:l + 1, piece * D:(piece + 1) * D].partition_broadcast(128),
              reads=[dkey('MODROW', l, l + 1, piece * D, (piece + 1) * D)])
        if gi is not None:
            P.dma('sp', g[:], K.gvec[names[gi]][l:l + 1, :].partition_broadcast(128), reads=[])
        if kind == 'scale':
            P.stt('dve', o[:], o[:], 1.0, g[:], ALU.add, ALU.mult)
        elif kind == 'mul':
            P.tt('dve', o[:], o[:], g[:], ALU.mult)


def rms_rstd(P, ss, n, eps, rstd):
    P.act(rstd, ss, AF.Sqrt, bias=float(eps), scale=1.0 / n)
    P.recip(rstd, rstd)


def make_stg(K, st, tag, n=2):
    return [st.enter_context(K.nc.sbuf_tensor(uname("%s_stg%d" % (tag, i)), [128, 1024], F32)) for i in range(n)]


def gen_load_cast_weight(K, wb, src3, kparts, ncols, stg, CW, ctr):
    P = K.P
    for k in range(kparts):
        P.dma('pool', wb[:, k, :], src3[k * 128:(k + 1) * 128, :], reads=[])
        yield


def load_cast_weight(K, st, name, src3, kparts, ncols, stg=None, CW=1024):
    nc, P = K.nc, K.P
    wb = st.enter_context(nc.sbuf_tensor(uname(name), [128, kparts, ncols], BF16))
    for _ in gen_load_cast_weight(K, wb, src3, kparts, ncols, None, CW, [0]):
        pass
    return wb


def norm_tile_to_hT(K, st_bufs, xt, bcA, bcB, hT, col0):
    P = K.P
    junk, ss, rstd, tmp, hb = st_bufs
    P.act(junk[:], xt, AF.Square, accum=ss[:, 0:1])
    rms_rstd(P, ss[:, 0:1], D, 1e-6, rstd[:, 0:1])
    P.stt('dve', tmp[:], xt, rstd[:, 0:1], bcA, ALU.mult, ALU.mult)
    P.tt('pool', hb[:], tmp[:], bcB, ALU.add)
    transpose_to(K, hb, hT, col0)


def transpose_to(K, hb, hT, col0):
    P = K.P
    psb = next_psT(K)
    for k in range(8):
        P.tr(psb[:, k * 128:(k + 1) * 128], hb[:, k * 128:(k + 1) * 128], K.identb[:])
    P.copy('act', hT[:, :, col0:col0 + 128], psb[:].rearrange("p (k t) -> p k t", k=8))


def phase_proj(K, l, xin):
    nc, P, S = K.nc, K.P, K.S
    with ExitStack() as st:
        sb = lambda n, s, d: st.enter_context(nc.sbuf_tensor(uname(n), list(s), d))
        phase_bcast(K, l, st, (0, 1))
        wb = load_cast_weight(K, st, "winb", K.w_in[l], 8, DIN)
        xts = [sb("a_x%d" % i, [128, D], F32) for i in range(2)]
        junk = sb("a_junk", [128, D], BF16)
        ss = sb("a_ss", [128, 1], F32)
        rstd = sb("a_rstd", [128, 1], F32)
        tmp = sb("a_tmp", [128, D], F32)
        hb = sb("a_hb", [128, D], BF16)
        hTs = [sb("a_hT%d" % i, [128, 8, 512], BF16) for i in range(2)]
        pos = [sb("a_po%d" % i, [128, 512], F32) for i in range(4)]
        tiles = [(c0, min(c0 + 128, DIN)) for c0 in range(0, DIN, 128)]
        ti = 0
        oi = 0
        for ch in range(K.NCH):
            hT = hTs[ch % 2]
            for t4 in range(4):
                tok0 = ch * 512 + t4 * 128
                xt = xts[ti % 2]
                ti += 1
                P.dma('sp', xt[:], xin[tok0:tok0 + 128, :], reads=[dkey(xin.tensor.name, tok0, tok0 + 128, 0, D)])
                norm_tile_to_hT(K, (junk, ss, rstd, tmp, hb), xt[:], K.bc[0][:], K.bc[1][:], hT, t4 * 128)
            for (c0, c1) in tiles:
                m = c1 - c0
                ps = next_ps(K)
                for k in range(8):
                    P.mm(ps[0:m, :], wb[:, k, c0:c1], hT[:, k, :], start=(k == 0), stop=(k == 7))
                po = pos[oi % 4]
                P.copy(('act', 'dve')[oi % 2], po[0:m, :], ps[0:m, :])
                P.dma('pool', K.PT[c0:c1, ch * 512:(ch + 1) * 512], po[0:m, :],
                      writes=[dkey('PT', c0, c1, ch * 512, (ch + 1) * 512)])
                oi += 1
        P.emit()


def phase_mixers(K, l):
    P = K.P
    if K.stub:
        for k in range(8):
            P.dma('sp', K.MIXT[k * 128:(k + 1) * 128, :], K.PT[k * 128:(k + 1) * 128, :],
                  reads=[dkey('PT', k * 128, (k + 1) * 128, 0, K.S)], writes=[dkey('MIXT', k * 128, (k + 1) * 128, 0, K.S)])
        P.emit()
        return
    if 'nsa' in K.only:
        phase_nsa(K, l)
    with ExitStack() as st:
        gens = []
        if 'rw' in K.only:
            gens.append(gen_rwkv2(K, l, st))
        if 'ml' in K.only:
            gens.append(gen_mlstm(K, l, st))
        run_gens(gens)
        P.emit()


def phase_out(K, l, xin, xmid, xout):
    nc, P, S = K.nc, K.P, K.S
    ost_ = ExitStack()
    sbo = lambda n, s, d: ost_.enter_context(nc.sbuf_tensor(uname(n), list(s), d))
    fstg = None
    wg = sbo("wgb", [128, 8, DFF], BF16)
    wu = sbo("wub", [128, 8, DFF], BF16)
    wd = sbo("wdb", [128, 22, D], BF16)
    ctr = [0]

    def chain():
        yield from gen_load_cast_weight(K, wg, K.ffn_g[l], 8, DFF, fstg, 512, ctr)
        yield from gen_load_cast_weight(K, wu, K.ffn_u[l], 8, DFF, fstg, 512, ctr)
        yield from gen_load_cast_weight(K, wd, K.ffn_d[l], 22, D, fstg, 512, ctr)
    wload = chain()

    def pull(n):
        for _ in range(n):
            try:
                next(wload)
            except StopIteration:
                return
    with ExitStack() as st:
        sb = lambda n, s, d: st.enter_context(nc.sbuf_tensor(uname(n), list(s), d))
        phase_bcast(K, l, st, (2,))
        wo = load_cast_weight(K, st, "woutb", K.w_out[l], 8, D)
        pull(1000)
        mT = [sb("o_mT%d" % i, [128, 8, 512], BF16) for i in range(2)]
        xts = [sb("o_x%d" % i, [128, D], F32) for i in range(1)]
        ys = [sb("o_y%d" % i, [128, D], F32) for i in range(2)]
        junk = sb("o_junk", [128, D], BF16)
        ss = sb("o_ss", [128, 1], F32)
        rstd = sb("o_rstd", [128, 1], F32)
        i = 0
        ti = 0

        def load_mix(ch):
            m = mT[ch % 2]
            for k in range(8):
                P.dma('pool', m[:, k, :], K.MIXT[k * 128:(k + 1) * 128, ch * 512:(ch + 1) * 512],
                      reads=[dkey('MIXT', k * 128, (k + 1) * 128, ch * 512, (ch + 1) * 512)])
        load_mix(0)
        for ch in range(K.NCH):
            m = mT[ch % 2]
            if ch + 1 < K.NCH:
                load_mix(ch + 1)
            for t4 in range(4):
                tok0 = ch * 512 + t4 * 128
                xt = xts[0]
                y = ys[ti % 2]
                ti += 1
                P.dma('sp', xt[:], xin[tok0:tok0 + 128, :], reads=[dkey(xin.tensor.name, tok0, tok0 + 128, 0, D)])
                for half in range(2):
                    ps = next_ps(K)
                    for k in range(8):
                        P.mm(ps[:, :], m[:, k, t4 * 128:(t4 + 1) * 128], wo[:, k, half * 512:(half + 1) * 512],
                             start=(k == 0), stop=(k == 7))
                    P.copy('act', y[:, half * 512:(half + 1) * 512], ps[:, :])
                P.act(junk[:], y[:], AF.Square, accum=ss[:, 0:1])
                rms_rstd(P, ss[:, 0:1], D, 1e-6, rstd[:, 0:1])
                P.stt('dve', y[:], y[:], rstd[:, 0:1], K.bc[2][:], ALU.mult, ALU.mult)
                P.tt('pool', y[:], y[:], xt[:], ALU.add)
                P.dma('pool', xmid[tok0:tok0 + 128, :], y[:], writes=[dkey(xmid.tensor.name, tok0, tok0 + 128, 0, D)])
        pull(1000)
        P.emit()
    with ExitStack() as st:
        sb = lambda n, s, d: st.enter_context(nc.sbuf_tensor(uname(n), list(s), d))
        phase_bcast(K, l, st, (3, 4, 5))
        xt = sb("f_x", [128, D], F32)
        junk = sb("f_junk", [128, D], BF16)
        ss = sb("f_ss", [128, 1], F32)
        rstd = sb("f_rstd", [128, 1], F32)
        hb = sb("f_hb", [128, D], BF16)
        hT = sb("f_hT", [128, 8, 512], BF16)
        sg = sb("f_sg", [128, 512], F32)
        aT = sb("f_aT", [128, 22, 512], BF16)
        y = sb("f_y", [128, D], F32)
        tmp = y
        for ch in range(K.NCH):
            for t4 in range(4):
                tok0 = ch * 512 + t4 * 128
                P.dma('sp', xt[:], xmid[tok0:tok0 + 128, :], reads=[dkey(xmid.tensor.name, tok0, tok0 + 128, 0, D)])
                norm_tile_to_hT(K, (junk, ss, rstd, tmp, hb), xt[:], K.bc[3][:], K.bc[4][:], hT, t4 * 128)
            for f in range(22):
                pg = next_ps(K)
                pu = next_ps(K)
                for k in range(8):
                    P.mm(pg[:, :], wg[:, k, f * 128:(f + 1) * 128], hT[:, k, :], start=(k == 0), stop=(k == 7))
                for k in range(8):
                    P.mm(pu[:, :], wu[:, k, f * 128:(f + 1) * 128], hT[:, k, :], start=(k == 0), stop=(k == 7))
                P.act(sg[:], pg[:, :], AF.Silu)
                P.tt('dve', aT[:, f, :], sg[:], pu[:, :], ALU.mult)
            for t4 in range(4):
                tok0 = ch * 512 + t4 * 128
                P.dma('sp', xt[:], xmid[tok0:tok0 + 128, :], reads=[dkey(xmid.tensor.name, tok0, tok0 + 128, 0, D)])
                for half in range(2):
                    ps = next_ps(K)
                    for f in range(22):
                        P.mm(ps[:, :], aT[:, f, t4 * 128:(t4 + 1) * 128], wd[:, f, half * 512:(half + 1) * 512], start=(f == 0), stop=(f == 21))
                    P.copy('act', y[:, half * 512:(half + 1) * 512], ps[:, :])
                P.act(junk[:], y[:], AF.Square, accum=ss[:, 0:1])
                rms_rstd(P, ss[:, 0:1], D, 1e-6, rstd[:, 0:1])
                P.stt('dve', y[:], y[:], rstd[:, 0:1], K.bc[5][:], ALU.mult, ALU.mult)
                P.tt('pool', y[:], y[:], xt[:], ALU.add)
                P.dma('pool', xout[tok0:tok0 + 128, :], y[:], writes=[dkey(xout.tensor.name, tok0, tok0 + 128, 0, D)])
        P.emit()
    ost_.close()


def make_in_maps(inputs, S, depth, nb):
    x = np.asarray(inputs["x"], np.float32)
    c = np.asarray(inputs["c"], np.float32)
    shared = {}
    for n in ("w_mod", "b_mod", "g_pre_mix", "g_post_mix", "g_pre_ffn", "g_post_ffn", "w_in", "w_out",
              "ffn_w_gate", "ffn_w_up", "ffn_w_down"):
        shared[n] = np.ascontiguousarray(np.asarray(inputs[n], np.float32)[:depth])
    shared["ident"] = np.eye(128, dtype=np.float32)
    shared["tri_le"] = np.triu(np.ones((128, 128), np.float32))
    f = lambda n: np.asarray(inputs[n], np.float32)[:depth]
    ncmp = S // 16 - 1
    NN = (ncmp + 127) // 128
    NS = S // 64
    n_ = np.arange(NN * 128)[:, None]
    q_ = np.arange(S)[None, :]
    cm = np.where((16 * n_ + 31 <= q_) & (n_ < ncmp), 0.0, NEGB).astype(np.float32)
    shared["c_cmpb"] = np.ascontiguousarray(cm.reshape(NN, 128, S))
    shared["c_E"] = (np.arange(S)[None, :] // 64 == np.arange(NS)[:, None]).astype(np.float32)
    k_ = np.arange(128)[:, None]
    qq = np.arange(128)[None, :]
    shared["c_caus"] = np.where(k_ <= qq, 0.0, NEGB).astype(np.float32)
    shared["c_wlow"] = np.where(k_ > qq, 0.0, NEGB).astype(np.float32)
    pos = np.arange(S)[:, None]
    j_ = np.arange(NS)[None, :]
    cur = pos // 64
    forced = (j_ == 0) | (j_ == cur) | (j_ == cur - 1)
    valid = j_ <= cur
    shared["c_vm"] = (valid & ~forced).astype(np.float32)
    shared["c_am"] = np.where(forced, 1000.0 + j_, np.where(valid, 0.0, -1.0 - j_)).astype(np.float32)
    cs_ = (np.arange(NN * 128) * 16)[:, None]
    ss_ = (np.arange(NS) * 64)[None, :]
    ov = ((cs_ < ss_ + 64) & (cs_ + 32 > ss_) & (np.arange(NN * 128)[:, None] < ncmp)).astype(np.float32)
    shared["c_ov"] = np.ascontiguousarray(ov.reshape(NN, 128, NS))
    mu = f("rw_mu")
    mua = np.concatenate([mu[:, :1152].reshape(depth, 18, 64), mu[:, 1152:1280].reshape(depth, 2, 64)], axis=1)
    shared["rw_mua"] = np.ascontiguousarray(mua.transpose(0, 2, 1))
    shared["rw_mug"] = np.ascontiguousarray(mu[:, 1280:1408].reshape(depth, 128, 1))
    vecs = [f(n).reshape(depth, 6, 64) for n in ("rw_w0", "rw_a0", "rw_kk", "rw_ka", "rw_ln_w", "rw_ln_b")] + [f("rw_rk")]
    shared["rw_vec"] = np.ascontiguousarray(np.stack(vecs, axis=1).transpose(0, 3, 1, 2))
    m2 = np.zeros((depth, 128, 10), np.float32)
    for q in range(3):
        for h in range(6):
            g, hh = divmod(h, 3)
            m2[:, 64 * g:64 * g + 64, q * 3 + hh] = mu[:, q * 384 + h * 64:q * 384 + (h + 1) * 64]
    m2[:, 0:64, 9] = mu[:, 1152:1216]
    m2[:, 64:128, 9] = mu[:, 1216:1280]
    shared["rw2_mua"] = m2
    v7 = np.stack(vecs, axis=1)
    shared["rw2_vec"] = np.ascontiguousarray(v7.reshape(depth, 7, 2, 3, 64).transpose(0, 2, 4, 1, 3).reshape(depth, 128, 7, 3))
    shared["rw_w2"] = f("rw_w2")
    shared["rw_a2"] = f("rw_a2")
    shared["rw_g2"] = f("rw_g2")
    s_ = np.arange(64)[:, None]
    t_ = np.arange(64)[None, :]
    m3 = np.stack([(s_ < t_), (s_ <= t_), (s_ > t_)]).astype(np.float32)
    shared["c_rmsk"] = np.ascontiguousarray(np.tile(m3.transpose(1, 0, 2)[:, :, None, :], (1, 1, 8, 1)).reshape(64, 3 * 8 * 64))
    shared["c_itile"] = np.ascontiguousarray(np.tile(np.eye(64, dtype=np.float32)[:, None, :], (1, 8, 1)).reshape(64, 512))
    shared["nsa_gate_b"] = f("nsa_gate_b")
    shared["nsa_out_g"] = f("nsa_out_g")
    shared["nsa_w1k"] = np.ascontiguousarray(f("nsa_ck_w1").reshape(depth, 32, 64, 64).transpose(0, 2, 1, 3))
    shared["nsa_w1v"] = np.ascontiguousarray(f("nsa_cv_w1").reshape(depth, 32, 64, 64).transpose(0, 2, 1, 3))
    shared["nsa_ck_w2"] = f("nsa_ck_w2")
    shared["nsa_cv_w2"] = f("nsa_cv_w2")
    shared["nsa_pekT"] = np.ascontiguousarray(f("nsa_pe_k").transpose(0, 2, 1))
    shared["nsa_pevT"] = np.ascontiguousarray(f("nsa_pe_v").transpose(0, 2, 1))
    cw = f("ml_conv_w")
    shared["ml_cw"] = np.ascontiguousarray(cw.transpose(0, 2, 1).reshape(depth, 12, 64, 4).transpose(0, 2, 1, 3))
    shared["ml_cb"] = np.ascontiguousarray(f("ml_conv_b").reshape(depth, 12, 64).transpose(0, 2, 1))
    shared["ml_gb"] = np.ascontiguousarray(np.concatenate([f("ml_ig_b"), f("ml_fg_b")], axis=1))
    shared["ml_norm_g"] = np.ascontiguousarray(f("ml_norm_g"))
    maps = []
    for b in range(nb):
        m = dict(shared)
        m["x"] = np.ascontiguousarray(x[b, :S])
        m["c"] = np.ascontiguousarray(c[b].reshape(8, 128).T)
        maps.append(m)
    return maps


_CACHE = {}


def kernel(**inputs):
    S, depth, nb = 4096, 2, 8
    if "prog" not in _CACHE:
        _CACHE["prog"] = build_program(S, depth)
    nc = _CACHE["prog"]
    maps = make_in_maps(inputs, S, depth, nb)
    res = run_bass_kernel_spmd(nc, maps, core_ids=list(range(nb)))
    return np.stack([np.asarray(r["out"], np.float32) for r in res.results], axis=0)


ML0 = 652 + 1408


def bl(ap2, n):
    p, m = ap2.shape
    return ap2.unsqueeze(2).to_broadcast([p, m, n])


ML_MS = 256


def gen_mlstm(K, l, st):
    nc, P, S = K.nc, K.P, K.S
    MS = ML_MS
    MC = MS // 128
    if True:
        sb = lambda n, s, d: st.enter_context(nc.sbuf_tensor(uname(n), list(s), d))
        cw = sb("ml_cw", [64, 12, 4], F32)
        cb = sb("ml_cb", [64, 12], F32)
        gb = sb("ml_gb", [128, 12], F32)
        ng = sb("ml_ng", [128, 384], F32)
        P.dma('sp', cw[:], K.ml_cw[l], reads=[])
        P.dma('sp', cb[:], K.ml_cb[l], reads=[])
        P.dma('sp', gb[:], K.ml_gb[l:l + 1, :].partition_broadcast(128), reads=[])
        P.dma('sp', ng[:], K.ml_ng[l:l + 1, :].partition_broadcast(128), reads=[])
        xin = [sb("ml_xin%d" % i, [64, 12, MS + 3], F32) for i in range(1)]
        xb = sb("ml_xb", [64, 12, MS + 3], BF16)
        Dg = sb("ml_Dg", [64, 12, 4, 64], BF16)
        for idx in range(12):
            for j in range(4):
                P.act(Dg[:, idx, j, :], K.identf[0:64, 0:64], AF.Copy, scale=cw[:, idx, j:j + 1])
        qkT = sb("ml_qkT", [64, 12, MS], BF16)
        vin = sb("ml_vin", [128, 3, MS], F32)
        vinb = sb("ml_vinb", [128, 3, MS], BF16)
        V1 = sb("ml_V1", [128, MC, 6, 65], BF16)
        oin = sb("ml_oin", [128, 3, MS], F32)
        OG = sb("ml_OG", [128, MC, 384], F32)
        gin = sb("ml_gin", [12, MS], F32)
        gsb = sb("ml_gsb", [128, MC, 12], F32)
        lf = sb("ml_lf", [128, MC, 6], F32)
        es = sb("ml_es", [128, MC, 6], F32)
        eb = sb("ml_eb", [128, MC, 6], F32)
        eg = sb("ml_eg", [128, MC, 6], F32)
        eh = sb("ml_eh", [128, MC, 6], F32)
        PsT = [sb("ml_PsT%d" % i, [128, 6, 128], BF16) for i in range(2)]
        Kh = [sb("ml_Kh%d" % i, [128, 6, 64], BF16) for i in range(2)]
        CT = sb("ml_CT", [64, 6, 65], F32)
        CTb = sb("ml_CTb", [64, 6, 65], BF16)
        dd = sb("ml_dd", [128, 6], F32)
        rr = sb("ml_rr", [128, 6], F32)
        hh = sb("ml_hh", [128, 6, 64], F32)
        sq = sb("ml_sq", [128, 6, 64], F32)
        ssum = sb("ml_ssum", [128, 6], F32)
        ho = sb("ml_ho", [128, 384], F32)
        ost = [sb("ml_ost%d" % i, [128, 3, MS], F32) for i in range(1)]
        P.memset('dve', CT[:], 0.0)
        P.memset('dve', CTb[:], 0.0)
        P.memset('pool', V1[:], 1.0)
        NSCm = S // MS
        for sc in range(NSCm):
            t0 = sc * MS
            xi = xin[0]
            for idx in range(12):
                r0 = ML0 + idx * 64
                if sc == 0:
                    P.memset('pool', xi[:, idx, 0:3], 0.0)
                    P.dma('sp', xi[:, idx, 3:MS + 3], K.PT[r0:r0 + 64, 0:MS], reads=[dkey('PT', r0, r0 + 64, 0, MS)])
                else:
                    P.dma('sp', xi[:, idx, :], K.PT[r0:r0 + 64, t0 - 3:t0 + MS], reads=[dkey('PT', r0, r0 + 64, t0 - 3, t0 + MS)])
            for i in range(3):
                r0 = ML0 + 768 + i * 128
                P.dma('pool', vin[:, i, :], K.PT[r0:r0 + 128, t0:t0 + MS], reads=[dkey('PT', r0, r0 + 128, t0, t0 + MS)])
                r0 = ML0 + 1152 + i * 128
                P.dma('pool', oin[:, i, :], K.PT[r0:r0 + 128, t0:t0 + MS], reads=[dkey('PT', r0, r0 + 128, t0, t0 + MS)])
            r0 = ML0 + 1536
            P.dma('sp', gin[:], K.PT[r0:r0 + 12, t0:t0 + MS], reads=[dkey('PT', r0, r0 + 12, t0, t0 + MS)])
            P.copy('act', xb[:], xi[:])
            for idx in range(12):
                ps = next_ps(K)
                for j in range(4):
                    P.mm(ps[0:64, 0:MS], Dg[:, idx, j, :], xb[:, idx, j:j + MS], start=(j == 0), stop=(j == 3))
                P.act(qkT[:, idx, :], ps[0:64, 0:MS], AF.Silu, bias=cb[:, idx:idx + 1])
            yield (sc + 0.1) / NSCm
            P.copy('pool', vinb[:], vin[:])
            P.act(oin[:], oin[:], AF.Sigmoid)
            for c in range(MC):
                pt = next_psT(K)
                for i in range(3):
                    P.tr(pt[:, i * 128:(i + 1) * 128], vinb[:, i, c * 128:(c + 1) * 128], K.identb[:])
                P.copy('act', V1[:, c, :, 0:64], pt[:, 0:384].rearrange("p (h e) -> p h e", h=6))
                ps = next_ps(K)
                for i in range(3):
                    P.tr(ps[:, i * 128:(i + 1) * 128], oin[:, i, c * 128:(c + 1) * 128], K.identf[:])
                P.copy('dve', OG[:, c, :], ps[:, 0:384])
                ps = next_ps(K)
                P.tr(ps[:, 0:12], gin[:, c * 128:(c + 1) * 128], K.identf[0:12, 0:12])
                P.tt('dve', gsb[:, c, :], ps[:, 0:12], gb[:], ALU.add)
            P.act(lf[:], gsb[:, :, 6:12], AF.Exp, scale=-1.0)
            P.act(lf[:], lf[:], AF.Ln, bias=1.0)
            P.ts('dve', lf[:], lf[:], -1.0, None, ALU.mult)
            for c in range(MC):
                ps = next_ps(K)
                P.mm(ps[:, 0:6], K.trif[:], lf[:, c, :])
                P.mm(ps[:, 8:14], K.onesf[:], lf[:, c, :], start=False)
                P.act(eb[:, c, :], ps[:, 0:6], AF.Exp)
                P.act(eg[:, c, :], ps[:, 8:14], AF.Exp)
                P.tt('dve', es[:, c, :], gsb[:, c, 0:6], ps[:, 0:6], ALU.subtract)
                P.act(es[:, c, :], es[:, c, :], AF.Exp, bias=-2.0794415416798357)
                P.tt('dve', eh[:, c, :], es[:, c, :], eg[:, c, :], ALU.mult)
            yield (sc + 0.2) / NSCm
            for c in range(MC):
                cs_ = slice(c * 128, (c + 1) * 128)
                pst = PsT[c % 2]
                kh = Kh[c % 2]
                psA = next_ps(K)
                psB = next_ps(K)
                for h in range(6):
                    pp = psA if h < 4 else psB
                    o = (h % 4) * 128
                    P.mm(pp[:, o:o + 128], qkT[:, 6 + h, cs_], qkT[:, h, cs_], start=(h % 4 == 0))
                for h in range(6):
                    pp = psA if h < 4 else psB
                    o = (h % 4) * 128
                    P.stt(('dve', 'pool')[h % 2] if False else 'dve', pst[:, h, :], pp[:, o:o + 128], es[:, c, h:h + 1], K.trif[:], ALU.mult, ALU.mult)
                pt = next_psT(K)
                for h in range(6):
                    P.tr(pt[:, h * 64:(h + 1) * 64], qkT[:, 6 + h, cs_], K.identb[0:64, 0:64])
                P.tt('dve', kh[:], pt[:, 0:384].rearrange("p (h e) -> p h e", h=6), bl(eh[:, c, :], 64), ALU.mult)
                pn = next_ps(K)
                for h in range(6):
                    P.mm(pn[:, h * 65:(h + 1) * 65], pst[:, h, :], V1[:, c, h, :], start=(h == 0))
                    P.mm(pn[:, h * 65:(h + 1) * 65], qkT[:, h, cs_], CTb[:, h, :], start=False)
                pc = next_ps(K)
                for h in range(6):
                    P.mm(pc[0:64, h * 65:(h + 1) * 65], kh[:, h, :], V1[:, c, h, :], start=(h == 0))
                P.tt('dve', CT[:], CT[:], bl(eg[0:64, c, :], 65), ALU.mult)
                P.tt('dve', CT[:], CT[:], pc[0:64, 0:390].rearrange("p (h e) -> p h e", h=6), ALU.add)
                P.copy('pool', CTb[:], CT[:])
                pn3 = pn[:, 0:390].rearrange("p (h e) -> p h e", h=6)
                P.act(dd[:], pn3[:, :, 64], AF.Abs)
                P.tt('dve', dd[:], dd[:], eb[:, c, :], ALU.mult)
                P.ts('dve', dd[:], dd[:], 1.0, None, ALU.max)
                P.recip(rr[:], dd[:])
                P.tt('dve', rr[:], rr[:], eb[:, c, :], ALU.mult)
                P.tt('dve', hh[:], pn3[:, :, 0:64], bl(rr[:], 64), ALU.mult)
                P.tt('pool', sq[:], hh[:], hh[:], ALU.mult)
                P.op('dve', lambda e: e.tensor_reduce(ssum[:], sq[:], AX.X, ALU.add), [sq[:]], [ssum[:]])
                P.act(ssum[:], ssum[:], AF.Sqrt, bias=1e-6, scale=1.0 / 64)
                P.recip(ssum[:], ssum[:])
                P.tt('dve', hh[:], hh[:], bl(ssum[:], 64), ALU.mult)
                ho3 = hh[:].rearrange("p h e -> p (h e)")
                P.tt('pool', ho[:], ho3, ng[:], ALU.mult)
                P.tt('dve', ho[:], ho[:], OG[:, c, :], ALU.mult)
                po = next_ps(K)
                for i in range(3):
                    P.tr(po[:, i * 128:(i + 1) * 128], ho[:, i * 128:(i + 1) * 128], K.identf[:])
                P.copy('act', ost[0][:, :, cs_], po[:, 0:384].rearrange("p (i t) -> p i t", i=3))
                yield (sc + 0.2 + 0.8 * (c + 1) / MC) / NSCm
            for i in range(3):
                r0 = 640 + i * 128
                P.dma('sp', K.MIXT[r0:r0 + 128, t0:t0 + MS], ost[0][:, i, :], writes=[dkey('MIXT', r0, r0 + 128, t0, t0 + MS)])


def run_gens(gens):
    prog = [0.0] * len(gens)
    live = list(range(len(gens)))
    while live:
        i = min(live, key=lambda k: prog[k])
        try:
            v = next(gens[i])
            prog[i] = v if v is not None else prog[i] + 1e-3
        except StopIteration:
            live.remove(i)


def phase_mlstm(K, l):
    with ExitStack() as st:
        run_gens([gen_mlstm(K, l, st)])
        K.P.emit()


def gelu_tanh(K, out_bf, x, t1, t2):
    P = K.P
    P.tt('dve', t1, x, x, ALU.mult)
    P.ts('dve', t1, t1, 0.044715, 1.0, ALU.mult, ALU.add)
    P.tt('dve', t1, t1, x, ALU.mult)
    P.act(t2, t1, AF.Sigmoid, scale=1.5957691216057308)
    P.tt('dve', out_bf, x, t2, ALU.mult)


def phase_nsa(K, l):
    nc, P, S, NT = K.nc, K.P, K.S, K.NT
    ncmp = S // 16 - 1
    NN = (ncmp + 127) // 128
    NS = S // 64
    CW = min(S, 2048)
    with ExitStack() as st:
        sb = lambda n, s, d: st.enter_context(nc.sbuf_tensor(uname(n), list(s), d))
        QT = sb("n_QT", [64, 4, S], BF16)
        kcT = sb("n_kcT", [64, S], BF16)
        vcT = sb("n_vcT", [64, S], BF16)
        ksT = sb("n_ksT", [64, S], BF16)
        kwT = sb("n_kwT", [64, S], BF16)
        vtmp = sb("n_vtmp", [64, S], BF16)
        V1s = sb("n_V1s", [128, NT, 65], BF16)
        V1w = sb("n_V1w", [128, NT, 65], BF16)
        G = sb("n_G", [128, NT, 12], F32)
        stg = [sb("n_stg%d" % i, [128, CW], F32) for i in range(3)]
        gin = sb("n_gin", [12, S], F32)
        gbias = sb("n_gbias", [128, 12], F32)
        gout = sb("n_gout", [128, 256], F32)
        cmpb = sb("n_cmpb", [128, NN, S], BF16)
        Eb = sb("n_Eb", [NS, S], BF16)
        causb = sb("n_causb", [128, 128], BF16)
        wlowb = sb("n_wlowb", [128, 128], BF16)
        vm = sb("n_vm", [128, NT, NS], F32)
        am = sb("n_am", [128, NT, NS], F32)
        OV = sb("n_OV", [128, NN, 64], BF16)
        VC1 = sb("n_VC1", [128, NN, 65], BF16)
        KcT = sb("n_KcT", [64, NN * 128], BF16)
        si = [0]

        def load_cast(dst, src, rows, dkeys, cols):
            for c0 in range(0, cols, CW):
                c1 = min(cols, c0 + CW)
                s_ = stg[si[0] % 3]
                si[0] += 1
                P.dma(('sp', 'pool')[si[0] % 2], s_[0:rows, 0:c1 - c0], src[:, c0:c1],
                      reads=[(dkeys[0], dkeys[1], dkeys[2], c0, c1)] if dkeys else [])
                P.copy(('dve', 'act')[si[0] % 2], dst[:, c0:c1], s_[0:rows, 0:c1 - c0])

        for h in range(4):
            load_cast(QT[:, h, :], K.PT[h * 64:(h + 1) * 64, :], 64, ('PT', h * 64, (h + 1) * 64), S)
        for dst, r0 in ((kcT, 256), (vcT, 320), (ksT, 384), (kwT, 512)):
            load_cast(dst[:, :], K.PT[r0:r0 + 64, :], 64, ('PT', r0, r0 + 64), S)
        for NNi in range(NN):
            load_cast(cmpb[:, NNi, :], K.c_cmpb[NNi], 128, None, S)
        load_cast(Eb[:, :], K.c_E[0:NS, 0:S], NS, None, S)
        load_cast(causb[:, :], K.c_caus, 128, None, 128)
        load_cast(wlowb[:, :], K.c_wlow, 128, None, 128)
        for NNi in range(NN):
            load_cast(OV[:, NNi, :], K.c_ov[NNi, :, 0:NS], 128, None, NS) if False else None
        P.dma('sp', vm[:], K.c_vm.rearrange("(t p) j -> p t j", p=128), reads=[])
        P.dma('sp', am[:], K.c_am.rearrange("(t p) j -> p t j", p=128), reads=[])
        P.dma('sp', gbias[:], K.nsa_gb[l:l + 1, :].partition_broadcast(128), reads=[])
        P.dma('sp', gout[:], K.nsa_og[l:l + 1, :].partition_broadcast(128), reads=[])
        P.memset('pool', V1s[:], 1.0)
        P.memset('pool', V1w[:], 1.0)
        P.memset('pool', VC1[:], 1.0)
        ovs = sb("n_ovs", [128, NN, NS], F32)
        P.dma('sp', ovs[:], K.c_ov.rearrange("n p j -> p n j"), reads=[])
        OVb = sb("n_OVb", [128, NN, NS], BF16)
        P.copy('dve', OVb[:], ovs[:])
        for (Vd, r0) in ((V1s, 448), (V1w, 576)):
            load_cast(vtmp[:, :], K.PT[r0:r0 + 64, :], 64, ('PT', r0, r0 + 64), S)
            for t8 in range(0, NT, 8):
                nt_ = min(8, NT - t8)
                pt = next_psT(K)
                for j in range(nt_):
                    P.tr(pt[:, j * 64:(j + 1) * 64], vtmp[:, (t8 + j) * 128:(t8 + j + 1) * 128], K.identb[0:64, 0:64])
                P.copy('act', Vd[:, t8:t8 + nt_, 0:64], pt[:, 0:nt_ * 64].rearrange("p (t e) -> p t e", t=nt_))
        P.dma('sp', gin[:], K.PT[640:652, :], reads=[dkey('PT', 640, 652, 0, S)])
        for t0_ in range(0, NT, 32):
            ps = next_ps(K)
            n_ = min(32, NT - t0_)
            for j in range(n_):
                P.tr(ps[:, j * 12:(j + 1) * 12], gin[:, (t0_ + j) * 128:(t0_ + j + 1) * 128], K.identf[0:12, 0:12])
            P.tt('dve', G[:, t0_:t0_ + n_, :], ps[:, 0:n_ * 12].rearrange("p (t g) -> p t g", t=n_),
                 gbias[:].unsqueeze(1).to_broadcast([128, n_, 12]), ALU.add)
        P.act(G[:], G[:], AF.Sigmoid)
        w1s = sb("n_w1s", [64, 32, 64], F32)
        w1b = sb("n_w1b", [64, 32, 64], BF16)
        w2s = sb("n_w2s", [64, 64], F32)
        w2b = sb("n_w2b", [64, 64], BF16)
        pes = sb("n_pes", [64, 32], F32)
        peb = sb("n_peb", [64, 32], BF16)
        c1 = sb("n_c1", [64, 1], F32)
        hx = sb("n_hx", [64, 512], F32)
        ht1 = sb("n_ht1", [64, 512], F32)
        ht2 = sb("n_ht2", [64, 512], F32)
        hidb = sb("n_hidb", [64, NN * 128], BF16)
        for which in range(2):
            src = (kcT, vcT)[which]
            P.dma('sp', w1s[:], K.nsa_w1[which][l], reads=[])
            P.dma('sp', w2s[:], K.nsa_w2[which][l], reads=[])
            P.dma('sp', pes[:], K.nsa_pe[which][l], reads=[])
            P.copy('dve', w1b[:], w1s[:])
            P.copy('dve', w2b[:], w2s[:])
            P.copy('dve', peb[:], pes[:])
            ps = next_ps(K)
            for j in range(32):
                P.mm(ps[0:64, 0:1], w1b[:, j, :], peb[:, j:j + 1], start=(j == 0), stop=(j == 31))
            P.copy('dve', c1[:], ps[0:64, 0:1])
            ps = next_ps(K)
            for j in range(32):
                P.mm(ps[0:64, 0:ncmp], w1b[:, j, :], src[:, j:j + 16 * (ncmp - 1) + 1:16], start=(j == 0), stop=(j == 31))
            P.act(hx[:, 0:ncmp], ps[0:64, 0:ncmp], AF.Identity, bias=c1[:, 0:1])
            P.memset('pool', hidb[:], 0.0)
            gelu_tanh(K, hidb[:, 0:ncmp], hx[:, 0:ncmp], ht1[:, 0:ncmp], ht2[:, 0:ncmp])
            if which == 0:
                ps = next_ps(K)
                P.mm(ps[0:64, 0:NN * 128], w2b[:], hidb[:])
                P.copy('dve', KcT[:], ps[0:64, 0:NN * 128])
            else:
                for nn in range(NN):
                    ps = next_ps(K)
                    P.mm(ps[:, 0:64], hidb[:, nn * 128:(nn + 1) * 128], w2b[:])
                    P.copy('dve', VC1[:, nn, 0:64], ps[:, 0:64])
        Pex = [sb("n_Pex%d" % i, [128, 512], BF16) for i in range(3)]
        pxi = [0]
        rden = sb("n_rden", [128, 4], F32)
        imp = sb("n_imp", [128, NS], F32)
        mx = sb("n_mx", [128, 8], F32)
        s2 = sb("n_s2", [128, NS], F32)
        s3 = sb("n_s3", [128, NS], F32)
        selb = sb("n_selb", [128, NS], BF16)
        selT = sb("n_selT", [NS, 128], BF16)
        dn = sb("n_dn", [128, 12], F32)
        oo = sb("n_oo", [128, 4, 64], F32)
        otmp = sb("n_otmp", [128, 4, 64], F32)
        junk = sb("n_junk", [128, 256], BF16)
        ss = sb("n_ss", [128, 1], F32)
        ost = [sb("n_ost%d" % i, [128, 2, 512], F32) for i in range(2)]
        pc, pu, psel, pw = K.ps[0], K.ps[1], K.ps[2], K.ps[3]
        sci = [0]

        def score_bank():
            b = K.ps[4 + sci[0] % 2]
            sci[0] += 1
            return b

        def score(blk):
            kT, V1, kt, q0, masks, acc, first, extra = blk
            ps = score_bank()
            P.mm(ps[:].rearrange("p (h q) -> p h q", h=4), kT[:, kt * 128:(kt + 1) * 128], QT[:, :, q0:q0 + 128], start=True, stop=False)
            for (ml, mr) in masks:
                P.mm(ps[:].rearrange("p (h q) -> p h q", h=4), ml, mr.unsqueeze(1).to_broadcast([mr.shape[0], 4, 128]), start=False, stop=False)
            px = Pex[pxi[0] % 3]
            pxi[0] += 1
            P.act(px[:], ps[:], AF.Exp, scale=0.125)
            return px

        def pv(blk, px):
            kT, V1, kt, q0, masks, acc, first, extra = blk
            for h in range(4):
                P.mm(acc[:, h * 65:(h + 1) * 65], px[:, h * 128:(h + 1) * 128], V1[:, kt, :], start=(first and h == 0), stop=False)
            if extra is not None:
                nn, a = extra
                for h in range(4):
                    P.mm(pu[:, h * NS:(h + 1) * NS], px[:, h * 128:(h + 1) * 128], OVb[:, nn, :], start=(a == 0 and h == 0), stop=False)

        def run_blocks(blks):
            prev = None
            for blk in blks:
                px = score(blk)
                if prev is not None:
                    pv(*prev)
                prev = (blk, px)
            if prev is not None:
                pv(*prev)

        for i in range(NT):
            q0 = i * 128
            nts = [nn for nn in range(NN) if nn * 2048 + 31 <= q0 + 127]
            blks = []
            for a, nn in enumerate(nts):
                blks.append((KcT, VC1, nn, q0, [(K.identb[:], cmpb[:, nn, q0:q0 + 128])], pc, a == 0, (nn, a)))
            run_blocks(blks)
            pc3 = pc[:, 0:260].rearrange("p (h e) -> p h e", h=4)
            P.ts('dve', rden[:], pc3[:, :, 64], 1e-30, None, ALU.max)
            P.recip(rden[:], rden[:])
            P.ts('dve', imp[:], pu[:, 0:NS], rden[:, 0:1], None, ALU.mult)
            for h in range(1, 4):
                P.stt('dve', imp[:], pu[:, h * NS:(h + 1) * NS], rden[:, h:h + 1], imp[:], ALU.mult, ALU.add)
            P.tt('dve', imp[:], imp[:], vm[:, i, :], ALU.mult)
            P.tt('dve', imp[:], imp[:], am[:, i, :], ALU.add)
            P.op('dve', lambda e: e.max(mx[:], imp[:]), [imp[:]], [mx[:]])
            P.op('dve', lambda e: e.match_replace(s2[:], mx[:], imp[:], -1e9), [mx[:], imp[:]], [s2[:]])
            P.op('dve', lambda e: e.max(mx[:], s2[:]), [s2[:]], [mx[:]])
            P.op('dve', lambda e: e.match_replace(s3[:], mx[:], s2[:], -1e9), [mx[:], s2[:]], [s3[:]])
            P.ts('dve', selb[:], s3[:], -1e8, NEGB, ALU.is_gt, ALU.mult)
            k0 = max(0, i - 4)
            blks = []
            for kt in range(k0, i + 1):
                masks = []
                if kt == i:
                    masks.append((K.identb[:], causb[:]))
                if kt == i - 4:
                    masks.append((K.identb[:], wlowb[:]))
                blks.append((kwT, V1w, kt, q0, masks, pw, kt == k0, None))
            run_blocks(blks)
            pt = next_psT(K)
            P.tr(pt[0:NS, 0:128], selb[:], K.identb[:])
            P.copy('dve', selT[:], pt[0:NS, 0:128])
            blks = []
            for kt in range(i + 1):
                masks = [(Eb[:, kt * 128:(kt + 1) * 128], selT[:])]
                if kt == i:
                    masks.append((K.identb[:], causb[:]))
                blks.append((ksT, V1s, kt, q0, masks, psel, kt == 0, None))
            run_blocks(blks)
            ps3 = psel[:, 0:260].rearrange("p (h e) -> p h e", h=4)
            pw3 = pw[:, 0:260].rearrange("p (h e) -> p h e", h=4)
            P.copy('dve', dn[:, 0:4], pc3[:, :, 64])
            P.copy('dve', dn[:, 4:8], ps3[:, :, 64])
            P.copy('dve', dn[:, 8:12], pw3[:, :, 64])
            P.ts('dve', dn[:], dn[:], 1e-30, None, ALU.max)
            P.recip(dn[:], dn[:])
            P.tt('dve', dn[:], dn[:], G[:, i, :], ALU.mult)
            P.tt('dve', oo[:], pc3[:, :, 0:64], bl(dn[:, 0:4], 64), ALU.mult)
            P.tt('dve', otmp[:], ps3[:, :, 0:64], bl(dn[:, 4:8], 64), ALU.mult)
            P.tt('pool', oo[:], oo[:], otmp[:], ALU.add)
            P.tt('dve', otmp[:], pw3[:, :, 0:64], bl(dn[:, 8:12], 64), ALU.mult)
            P.tt('pool', oo[:], oo[:], otmp[:], ALU.add)
            o2 = oo[:].rearrange("p h e -> p (h e)")
            P.act(junk[:], o2, AF.Square, accum=ss[:, 0:1])
            rms_rstd(P, ss[:, 0:1], 256, 1e-6, ss[:, 0:1])
            P.stt('dve', o2, o2, ss[:, 0:1], gout[:], ALU.mult, ALU.mult)
            po = next_psT(K) if False else None
            pso = K.ps[4 + sci[0] % 2]
            sci[0] += 1
            for j in range(2):
                P.tr(pso[:, j * 128:(j + 1) * 128], o2[:, j * 128:(j + 1) * 128], K.identf[:])
            os_ = ost[(i // 4) % 2]
            P.copy('act', os_[:, :, (i % 4) * 128:(i % 4 + 1) * 128], pso[:, 0:256].rearrange("p (j t) -> p j t", j=2))
            if i % 4 == 3:
                t0 = (i // 4) * 512
                for j in range(2):
                    P.dma('sp', K.MIXT[j * 128:(j + 1) * 128, t0:t0 + 512], os_[:, j, :], writes=[dkey('MIXT', j * 128, (j + 1) * 128, t0, t0 + 512)])
        P.emit()


RW0 = 652
RW_TS = 256
RW_NSETS = 1


def gen_rwkv(K, l, st):
    nc, P, S = K.nc, K.P, K.S
    TS = RW_TS
    NC = TS // 64
    HB = 512 // TS
    if True:
        sb = lambda n, s, d: st.enter_context(nc.sbuf_tensor(uname(n), list(s), d))
        sets = []
        for bi in range(RW_NSETS):
            farena = sb("r_far%d" % bi, [64, 12, 6, TS], F32)
            barena = sb("r_bar%d" % bi, [64, 16, 6, TS], BF16)
            f2d = farena[:].rearrange("p s h t -> p (s h t)")
            sets.append(dict(
                F=[farena[:, k] for k in range(12)], B=[barena[:, k] for k in range(16)],
                X=f2d[:, 0:20 * (TS + 1)].rearrange("p (i t) -> p i t", i=20),
                Dl=f2d[:, 4 * 6 * TS:4 * 6 * TS + 20 * TS].rearrange("p (i t) -> p i t", i=20),
                XG=sb("r_XG%d" % bi, [128, TS + 1], F32), sgx=sb("r_sgx%d" % bi, [128, TS], F32),
                sgxb=sb("r_sgxb%d" % bi, [128, TS], BF16),
                Zs=sb("r_Zs%d" % bi, [64, 6, 64], BF16), Us=sb("r_Us%d" % bi, [64, 6, 64], BF16),
                WL=sb("r_WL%d" % bi, [64, 6, NC], F32)))
        mua = sb("r_mua", [64, 20], F32)
        mug = sb("r_mug", [128, 1], F32)
        vec = sb("r_vec", [64, 7, 6], F32)
        omka = sb("r_omka", [64, 6], F32)
        wst = sb("r_wst", [128, 384], F32)
        w2 = sb("r_w2", [64, 384], BF16)
        a2 = sb("r_a2", [64, 384], BF16)
        g2 = sb("r_g2", [128, 384], BF16)
        onesb = sb("r_onesb", [64, 64], BF16)
        rm = sb("r_rm", [64, TS], F32)
        mskf = sb("r_mskf", [64, 3, 8, 64], F32)
        msk = sb("r_msk", [64, 3, HB, NC, 64], F32)
        itl = sb("r_itl", [64, HB, NC, 64], F32)
        ST = sb("r_ST", [64, 6, 64], F32)
        STb = sb("r_STb", [64, 6, 64], BF16)
        P.dma('sp', mua[:], K.rw_mua[l], reads=[])
        P.dma('sp', mug[:], K.rw_mug[l], reads=[])
        P.dma('sp', vec[:], K.rw_vec[l], reads=[])
        for (dst, src, rows) in ((w2, K.rw_w2[l], 64), (a2, K.rw_a2[l], 64), (g2, K.rw_g2[l], 128)):
            P.dma('sp', wst[0:rows, :], src, reads=[])
            P.copy('dve', dst[:], wst[0:rows, :])
        P.dma('sp', mskf[:].rearrange("p m c t -> p (m c t)"), K.c_rmsk, reads=[])
        for m in range(3):
            for hb in range(HB):
                P.copy('pool', msk[:, m, hb], mskf[:, m, 0:NC, :])
        for hb in range(HB):
            P.copy('pool', itl[:, hb].rearrange("p c t -> p (c t)"), K.c_itile_sb[:, 0:NC * 64])
        P.ts('dve', omka[:], vec[:, 3, :], -1.0, 1.0, ALU.mult, ALU.add)
        P.memset('dve', rm[:], 1.0)
        P.memset('dve', rm[:, 0:TS:64], 0.0)
        P.memset('dve', ST[:], 0.0)
        P.memset('dve', STb[:], 0.0)
        P.memset('pool', onesb[:], 1.0)
        W0, A0, KK, KA, LNW, LNB, RK = range(7)
        i64 = K.identb[0:64, 0:64]

        def hgroups():
            for h0 in range(0, 6, HB):
                yield h0, min(HB, 6 - h0)

        def mm_blocks(dst_fn, lt, rt, evac):
            for h0, nh in hgroups():
                ps = next_ps(K)
                first = True
                for j in range(nh):
                    for c in range(NC):
                        cc = slice(c * 64, (c + 1) * 64)
                        o = (j * NC + c) * 64
                        P.mm(ps[0:64, o:o + 64], lt[:, h0 + j, cc], rt[:, h0 + j, cc], start=first)
                        first = False
                evac(h0, nh, ps[0:64, 0:nh * TS])

        def superchunk(sc, bs):
            F, B, X, Dl, XG, sgx, sgxb, Zs, Us, WL = (bs[k] for k in ("F", "B", "X", "Dl", "XG", "sgx", "sgxb", "Zs", "Us", "WL"))
            t0 = sc * TS
            for idx in range(20):
                r0 = RW0 + idx * 64
                if sc == 0:
                    P.memset('pool', X[:, idx, 0:1], 0.0)
                    P.dma(('sp', 'pool')[idx % 2], X[:, idx, 1:TS + 1], K.PT[r0:r0 + 64, 0:TS], reads=[dkey('PT', r0, r0 + 64, 0, TS)])
                else:
                    P.dma(('sp', 'pool')[idx % 2], X[:, idx, :], K.PT[r0:r0 + 64, t0 - 1:t0 + TS], reads=[dkey('PT', r0, r0 + 64, t0 - 1, t0 + TS)])
            r0 = RW0 + 1280
            if sc == 0:
                P.memset('pool', XG[:, 0:1], 0.0)
                P.dma('sp', XG[:, 1:TS + 1], K.PT[r0:r0 + 128, 0:TS], reads=[dkey('PT', r0, r0 + 128, 0, TS)])
            else:
                P.dma('sp', XG[:, :], K.PT[r0:r0 + 128, t0 - 1:t0 + TS], reads=[dkey('PT', r0, r0 + 128, t0 - 1, t0 + TS)])
            P.tt('dve', Dl, X[:, :, 0:TS], X[:, :, 1:TS + 1], ALU.subtract)
            P.tt('dve', Dl, Dl, bl(mua[:], TS), ALU.mult)
            P.tt('pool', Dl, Dl, X[:, :, 1:TS + 1], ALU.add)
            P.tt('dve', sgx[:], XG[:, 0:TS], XG[:, 1:TS + 1], ALU.subtract)
            P.stt('dve', sgx[:], sgx[:], mug[:, 0:1], XG[:, 1:TS + 1], ALU.mult, ALU.add)
            P.act(sgxb[:], sgx[:], AF.Sigmoid)
            yield
            rp, kp, vp = F[4], F[5], F[6]
            xw, xa = F[7][:, 0, :], F[7][:, 1, :]
            twb, xab = B[0][:, 0, :], B[0][:, 1, :]
            P.act(twb, xw, AF.Tanh)
            P.copy('act', xab, xa)
            ld, aa, gg, kk, kmod = F[0], F[1], F[8], F[2], F[3]
            for (wt, rhs, dst, func, bi) in ((w2, twb, ld, AF.Sigmoid, W0), (a2, xab, aa, AF.Sigmoid, A0), (g2, sgxb[:], gg, None, None)):
                for h0, nh in hgroups():
                    ps = next_ps(K)
                    for j in range(nh):
                        h = h0 + j
                        P.mm(ps[0:64, j * TS:(j + 1) * TS], wt[:, h * 64:(h + 1) * 64], rhs, start=(j == 0))
                    for j in range(nh):
                        h = h0 + j
                        if func is None:
                            P.copy('act', dst[:, h, :], ps[0:64, j * TS:(j + 1) * TS])
                        else:
                            P.act(dst[:, h, :], ps[0:64, j * TS:(j + 1) * TS], func, bias=vec[:, bi, h:h + 1])
            yield
            P.ts('dve', ld[:], ld[:], -0.6065306597126334, None, ALU.mult)
            kk2b = B[1]
            P.tt('dve', kk[:], kp[:], bl(vec[:, KK, :], TS), ALU.mult)
            P.tt('pool', kk2b[:], kk[:], kk[:], ALU.mult)
            for h0, nh in hgroups():
                ps = next_ps(K)
                for j in range(nh):
                    P.mm(ps[0:64, j * TS:(j + 1) * TS], onesb[:], kk2b[:, h0 + j, :], start=(j == 0))
                P.act(F[9][:, h0:h0 + nh, :], ps[0:64, 0:nh * TS].rearrange("p (h t) -> p h t", h=nh), AF.Sqrt)
            P.ts('dve', F[9][:], F[9][:], 1e-12, None, ALU.max)
            P.recip(F[9][:], F[9][:])
            P.tt('dve', kk[:], kk[:], F[9][:], ALU.mult)
            yield
            P.tt('dve', kmod[:], aa[:], bl(vec[:, KA, :], TS), ALU.mult)
            P.tt('dve', kmod[:], kmod[:], bl(omka[:], TS), ALU.add)
            P.tt('pool', kmod[:], kmod[:], kp[:], ALU.mult)
            yield
            cl, eW, eWm = F[9], F[10], F[11]
            for h in range(6):
                P.op('dve', lambda e, h=h: e.tensor_tensor_scan(cl[:, h, :], rm[:], ld[:, h, :], 0.0, ALU.mult, ALU.add),
                     [rm[:], ld[:, h, :]], [cl[:, h, :]])
            P.act(eW[:], cl[:], AF.Exp)
            P.tt('dve', eWm[:], cl[:], ld[:], ALU.subtract)
            P.act(eWm[:], eWm[:], AF.Exp)
            P.copy('act', WL[:], eW[:, :, 63:TS:64])
            yield
            At, Rt, Bt, Kt = B[2], B[3], B[4], B[5]
            P.stt('dve', At[:], eWm[:], -1.0, kk[:], ALU.mult, ALU.mult)
            P.tt('pool', Rt[:], rp[:], eW[:], ALU.mult)
            P.act(cl[:], cl[:], AF.Exp, scale=-1.0)
            P.tt('dve', eWm[:], kk[:], aa[:], ALU.mult)
            P.tt('dve', Bt[:], eWm[:], cl[:], ALU.mult)
            P.tt('pool', Kt[:], kmod[:], cl[:], ALU.mult)
            yield
            BV = F[0]
            rkrb = B[1]
            P.tt('dve', eWm[:], rp[:], kmod[:], ALU.mult)
            P.tt('dve', rkrb[:], eWm[:], bl(vec[:, RK, :], TS), ALU.mult)
            for h0, nh in hgroups():
                ps = next_ps(K)
                for j in range(nh):
                    P.mm(ps[0:64, j * TS:(j + 1) * TS], onesb[:], rkrb[:, h0 + j, :], start=(j == 0))
                P.tt('dve', BV[:, h0:h0 + nh, :], ps[0:64, 0:nh * TS].rearrange("p (h t) -> p h t", h=nh), vp[:, h0:h0 + nh, :], ALU.mult)
            vpb = B[0]
            P.copy('act', vpb[:], vp[:])
            yield
            Vtok, Btok, Ktok = B[6], B[7], B[8]
            ei = 0
            for (src, dst) in ((vpb, Vtok), (Bt, Btok), (Kt, Ktok)):
                for h0 in range(0, 6, 3):
                    pt = next_psT(K)
                    for j in range(3):
                        for c in range(NC):
                            o = (j * NC + c) * 64
                            P.tr(pt[0:64, o:o + 64], src[:, h0 + j, c * 64:(c + 1) * 64], i64)
                    P.copy('act', dst[:, h0:h0 + 3, :], pt[0:64, 0:3 * TS].rearrange("p (h t) -> p h t", h=3))
                    ei += 1
            yield
            NTm, Nm, Arb, Aak, Ark = B[9], B[10], B[11], B[12], B[13]
            for (dst, lt, rt, mi) in ((NTm, Bt, At, 0), (Nm, At, Bt, 2), (Arb, Bt, Rt, 1), (Aak, Kt, At, 0), (Ark, Kt, Rt, 1)):
                def ev(h0, nh, pv, dst=dst, mi=mi):
                    P.tt('dve', dst[:, h0:h0 + nh, :], pv.rearrange("p (h t) -> p h t", h=nh),
                         msk[:, mi, 0:nh].rearrange("p h c t -> p h (c t)"), ALU.mult)
                mm_blocks(None, lt, rt, ev)
                yield
            yield
            Tt32, Ttb = F[1], B[14]
            for h0, nh in hgroups():
                P.tt('dve', Tt32[:, h0:h0 + nh, :], NTm[:, h0:h0 + nh, :], itl[:, 0:nh].rearrange("p h c t -> p h (c t)"), ALU.add)
            P.copy('act', Ttb[:], Tt32[:])
            cN, cNT, nN, nNT = Nm, NTm, B[15], B[1]
            for lvl in range(5):
                last = (lvl == 4)
                def evN(h0, nh, pv, d=nN):
                    P.copy('act', d[:, h0:h0 + nh, :], pv.rearrange("p (h t) -> p h t", h=nh))
                mm_blocks(None, cNT, cN, evN)
                if not last:
                    def evNT(h0, nh, pv, d=nNT):
                        P.copy('act', d[:, h0:h0 + nh, :], pv.rearrange("p (h t) -> p h t", h=nh))
                    mm_blocks(None, cN, cNT, evNT)
                def evT(h0, nh, pv):
                    P.tt('dve', Tt32[:, h0:h0 + nh, :], Tt32[:, h0:h0 + nh, :], pv.rearrange("p (h t) -> p h t", h=nh), ALU.add)
                    P.copy('act', Ttb[:, h0:h0 + nh, :], Tt32[:, h0:h0 + nh, :])
                mm_blocks(None, nN, Ttb, evT)
                cN, cNT, nN, nNT = nN, nNT, cN, cNT
                yield
            yield
            Yt = F[2]
            for c in range(NC):
                cc = slice(c * 64, (c + 1) * 64)
                pz = next_ps(K)
                for h in range(6):
                    o = slice(h * 64, (h + 1) * 64)
                    P.mm(pz[0:64, o], At[:, h, cc], STb[:, h, :], start=(h == 0))
                    P.mm(pz[0:64, o], Aak[:, h, cc], Vtok[:, h, cc], start=False)
                P.copy('act', Zs[:].rearrange("p h v -> p (h v)"), pz[0:64, 0:384])
                pu_ = next_ps(K)
                for h in range(6):
                    o = slice(h * 64, (h + 1) * 64)
                    P.mm(pu_[0:64, o], Ttb[:, h, cc], Zs[:, h, :], start=(h == 0))
                P.copy('act', Us[:].rearrange("p h v -> p (h v)"), pu_[0:64, 0:384])
                py = next_ps(K)
                for h in range(6):
                    o = slice(h * 64, (h + 1) * 64)
                    P.mm(py[0:64, o], STb[:, h, :], Rt[:, h, cc], start=(h == 0))
                    P.mm(py[0:64, o], Us[:, h, :], Arb[:, h, cc], start=False)
                    P.mm(py[0:64, o], Vtok[:, h, cc], Ark[:, h, cc], start=False)
                P.copy('act', Yt[:, :, cc], py[0:64, 0:384].rearrange("p (h t) -> p h t", h=6))
                pn = next_ps(K)
                for h in range(6):
                    o = slice(h * 64, (h + 1) * 64)
                    P.mm(pn[0:64, o], Btok[:, h, cc], Us[:, h, :], start=(h == 0))
                    P.mm(pn[0:64, o], Ktok[:, h, cc], Vtok[:, h, cc], start=False)
                P.tt('dve', ST[:], ST[:], pn[0:64, 0:384].rearrange("p (h v) -> p h v", h=6), ALU.add)
                P.tt('dve', ST[:], ST[:], bl(WL[:, :, c], 64), ALU.mult)
                P.copy('act', STb[:], ST[:])
                yield
            yield
            yc, sq = F[3], F[5]
            Ytb, sqb = B[4], B[5]
            P.copy('act', Ytb[:], Yt[:])
            for h0, nh in hgroups():
                ps = next_ps(K)
                for j in range(nh):
                    P.mm(ps[0:64, j * TS:(j + 1) * TS], onesb[:], Ytb[:, h0 + j, :], start=(j == 0))
                P.stt('dve', yc[:, h0:h0 + nh, :], ps[0:64, 0:nh * TS].rearrange("p (h t) -> p h t", h=nh), -1.0 / 64,
                      Yt[:, h0:h0 + nh, :], ALU.mult, ALU.add)
            P.tt('pool', sqb[:], yc[:], yc[:], ALU.mult)
            for h0, nh in hgroups():
                ps = next_ps(K)
                for j in range(nh):
                    P.mm(ps[0:64, j * TS:(j + 1) * TS], onesb[:], sqb[:, h0 + j, :], start=(j == 0))
                P.act(sq[:, h0:h0 + nh, :], ps[0:64, 0:nh * TS].rearrange("p (h t) -> p h t", h=nh), AF.Sqrt, bias=64e-5, scale=1.0 / 64)
            P.recip(sq[:], sq[:])
            P.tt('dve', yc[:], yc[:], sq[:], ALU.mult)
            P.tt('dve', yc[:], yc[:], bl(vec[:, LNW, :], TS), ALU.mult)
            P.tt('pool', yc[:], yc[:], bl(vec[:, LNB, :], TS), ALU.add)
            P.tt('pool', yc[:], yc[:], BV[:], ALU.add)
            P.tt('dve', yc[:], yc[:], gg[:], ALU.mult)
            for h in range(6):
                r0 = 256 + h * 64
                P.dma(('sp', 'pool')[h % 2], K.MIXT[r0:r0 + 64, t0:t0 + TS], yc[:, h, :], writes=[dkey('MIXT', r0, r0 + 64, t0, t0 + TS)])

        NSC = S // TS
        LAG = 14
        active = []
        nxt = 0
        while nxt < NSC or active:
            if nxt < NSC and len(active) < RW_NSETS and (not active or active[-1][1] >= LAG):
                active.append([superchunk(nxt, sets[nxt % RW_NSETS]), 0])
                nxt += 1
            for a in list(active):
                try:
                    next(a[0])
                    a[1] += 1
                except StopIteration:
                    active.remove(a)
            yield


def phase_rwkv(K, l):
    with ExitStack() as st:
        for _ in gen_rwkv(K, l, st):
            pass
        K.P.emit()


RW2_NSETS = 2
RW2_LAG = 12


def gen_rwkv2(K, l, st):
    nc, P, S = K.nc, K.P, K.S
    TS = 256
    NC = TS // 64
    sb = lambda n, s, d: st.enter_context(nc.sbuf_tensor(uname(n), list(s), d))
    sets = []
    for bi in range(RW2_NSETS):
        farena = sb("q_far%d" % bi, [128, 12, 3, TS], F32)
        barena = sb("q_bar%d" % bi, [128, 16, 3, TS], BF16)
        f2d = farena[:].rearrange("p s h t -> p (s h t)")
        sets.append(dict(
            F=[farena[:, k] for k in range(12)], B=[barena[:, k] for k in range(16)],
            X=f2d[:, 0:10 * (TS + 1)].rearrange("p (i t) -> p i t", i=10),
            Dl=f2d[:, 4 * 3 * TS:4 * 3 * TS + 10 * TS].rearrange("p (i t) -> p i t", i=10),
            XG=sb("q_XG%d" % bi, [128, TS + 1], F32), sgx=sb("q_sgx%d" % bi, [128, TS], F32),
            sgxb=sb("q_sgxb%d" % bi, [128, TS], BF16), Zs=sb("q_Zs%d" % bi, [128, 3, 64], BF16),
            Us=sb("q_Us%d" % bi, [128, 3, 64], BF16), WL=sb("q_WL%d" % bi, [128, 3, NC], F32)))
    mua = sb("q_mua", [128, 10], F32)
    mug = sb("q_mug", [128, 1], F32)
    vec = sb("q_vec", [128, 7, 3], F32)
    omka = sb("q_omka", [128, 3], F32)
    wst = sb("q_wst", [128, 384], F32)
    wa2 = sb("q_wa2", [128, 384], BF16)
    g2 = sb("q_g2", [128, 384], BF16)
    bd = sb("q_bd", [128, 128], BF16)
    rm = sb("q_rm", [128, TS], F32)
    msk = sb("q_msk", [128, 3, 2, NC, 64], F32)
    itl = sb("q_itl", [128, 2, NC, 64], F32)
    ST = sb("q_ST", [128, 3, 64], F32)
    STb = sb("q_STb", [128, 3, 64], BF16)
    P.dma('sp', mua[:], K.rw2_mua[l], reads=[])
    P.dma('sp', mug[:], K.rw_mug[l], reads=[])
    P.dma('sp', vec[:], K.rw2_vec[l], reads=[])
    P.dma('sp', wst[0:64, :], K.rw_w2[l], reads=[])
    P.dma('sp', wst[64:128, :], K.rw_a2[l], reads=[])
    P.copy('dve', wa2[:], wst[:])
    P.dma('sp', wst[:], K.rw_g2[l], reads=[])
    P.copy('dve', g2[:], wst[:])
    rmv = K.c_rmsk.rearrange("p (m c t) -> p m c t", m=3, c=8)
    for g in range(2):
        for m in range(3):
            for j in range(2):
                P.dma('sp', msk[64 * g:64 * g + 64, m, j], rmv[:, m, 0:NC, :], reads=[])
    for g in range(2):
        for j in range(2):
            P.dma('sp', itl[64 * g:64 * g + 64, j].rearrange("p c t -> p (c t)"), K.c_itile[:, 0:NC * 64], reads=[])
    P.ts('dve', omka[:], vec[:, 3, :], -1.0, 1.0, ALU.mult, ALU.add)
    P.memset('dve', rm[:], 1.0)
    P.memset('dve', rm[:, 0:TS:64], 0.0)
    P.memset('dve', ST[:], 0.0)
    P.memset('dve', STb[:], 0.0)
    P.memset('pool', bd[:], 0.0)
    P.memset('pool', bd[0:64, 0:64], 1.0)
    P.memset('pool', bd[64:128, 64:128], 1.0)
    W0, A0, KK, KA, LNW, LNB, RK = range(7)
    GR = ((0, 2), (2, 1))

    def hp(g):
        return slice(64 * g, 64 * g + 64)

    def mm_blocks(lt, rt, evac):
        for hh0, n in GR:
            ps = next_ps(K)
            for g in range(2):
                first = True
                for j in range(n):
                    for c in range(NC):
                        cc = slice(c * 64, (c + 1) * 64)
                        o = (j * NC + c) * 64
                        P.mm(ps[hp(g), o:o + 64], lt[hp(g), hh0 + j, cc], rt[hp(g), hh0 + j, cc], start=first)
                        first = False
            evac(hh0, n, ps[:, 0:n * TS].rearrange("p (h t) -> p h t", h=n))

    def colsum(dst_fn, src):
        for hh0, n in GR:
            ps = next_ps(K)
            for j in range(n):
                P.mm(ps[:, j * TS:(j + 1) * TS], bd[:], src[:, hh0 + j, :], start=(j == 0))
            dst_fn(hh0, n, ps[:, 0:n * TS].rearrange("p (h t) -> p h t", h=n))

    def superchunk(sc, bs):
        F, B, X, Dl, XG, sgx, sgxb, Zs, Us, WL = (bs[k] for k in ("F", "B", "X", "Dl", "XG", "sgx", "sgxb", "Zs", "Us", "WL"))
        t0 = sc * TS
        di = 0
        for q in range(3):
            for h in range(6):
                g, hh = divmod(h, 3)
                r0 = RW0 + q * 384 + h * 64
                dst = X[hp(g), q * 3 + hh, :]
                eng = ('sp', 'pool')[di % 2]
                di += 1
                if sc == 0:
                    P.memset('pool', dst[:, 0:1], 0.0)
                    P.dma(eng, dst[:, 1:TS + 1], K.PT[r0:r0 + 64, 0:TS], reads=[dkey('PT', r0, r0 + 64, 0, TS)])
                else:
                    P.dma(eng, dst, K.PT[r0:r0 + 64, t0 - 1:t0 + TS], reads=[dkey('PT', r0, r0 + 64, t0 - 1, t0 + TS)])
        for g in range(2):
            r0 = RW0 + 1152 + 64 * g
            dst = X[hp(g), 9, :]
            if sc == 0:
                P.memset('pool', dst[:, 0:1], 0.0)
                P.dma('sp', dst[:, 1:TS + 1], K.PT[r0:r0 + 64, 0:TS], reads=[dkey('PT', r0, r0 + 64, 0, TS)])
            else:
                P.dma('sp', dst, K.PT[r0:r0 + 64, t0 - 1:t0 + TS], reads=[dkey('PT', r0, r0 + 64, t0 - 1, t0 + TS)])
        r0 = RW0 + 1280
        if sc == 0:
            P.memset('pool', XG[:, 0:1], 0.0)
            P.dma('sp', XG[:, 1:TS + 1], K.PT[r0:r0 + 128, 0:TS], reads=[dkey('PT', r0, r0 + 128, 0, TS)])
        else:
            P.dma('sp', XG[:, :], K.PT[r0:r0 + 128, t0 - 1:t0 + TS], reads=[dkey('PT', r0, r0 + 128, t0 - 1, t0 + TS)])
        P.tt('dve', Dl, X[:, :, 0:TS], X[:, :, 1:TS + 1], ALU.subtract)
        P.tt('dve', Dl, Dl, bl(mua[:], TS), ALU.mult)
        P.tt('pool', Dl, Dl, X[:, :, 1:TS + 1], ALU.add)
        P.tt('dve', sgx[:], XG[:, 0:TS], XG[:, 1:TS + 1], ALU.subtract)
        P.stt('dve', sgx[:], sgx[:], mug[:, 0:1], XG[:, 1:TS + 1], ALU.mult, ALU.add)
        P.act(sgxb[:], sgx[:], AF.Sigmoid)
        yield
        rp, kp, vp = F[4], F[5], F[6]
        txb = B[0][:, 0, :]
        P.act(txb[0:64], F[7][0:64, 0, :], AF.Tanh)
        P.copy('act', txb[64:128], F[7][64:128, 0, :])
        ld, aa, gg, kk, kmod = F[0], F[1], F[8], F[2], F[3]
        for (kind, dst, bi) in (('w', ld, W0), ('a', aa, A0), ('g', gg, None)):
            for hh0, n in GR:
                ps = next_ps(K)
                for g in range(2):
                    for j in range(n):
                        h = 3 * g + hh0 + j
                        hs = slice(h * 64, (h + 1) * 64)
                        o = ps[hp(g), j * TS:(j + 1) * TS]
                        if kind == 'w':
                            P.mm(o, wa2[0:64, hs], txb[0:64], start=(j == 0))
                        elif kind == 'a':
                            P.mm(o, wa2[64:128, hs], txb[64:128], start=(j == 0))
                        else:
                            P.mm(o, g2[:, hs], sgxb[:], start=(j == 0))
                for j in range(n):
                    hh = hh0 + j
                    if bi is None:
                        P.copy('act', dst[:, hh, :], ps[:, j * TS:(j + 1) * TS])
                    else:
                        P.act(dst[:, hh, :], ps[:, j * TS:(j + 1) * TS], AF.Sigmoid, bias=vec[:, bi, hh:hh + 1])
        yield
        P.ts('dve', ld[:], ld[:], -0.6065306597126334, None, ALU.mult)
        kk2b = B[1]
        P.tt('dve', kk[:], kp[:], bl(vec[:, KK, :], TS), ALU.mult)
        P.tt('pool', kk2b[:], kk[:], kk[:], ALU.mult)
        colsum(lambda hh0, n, pv: P.act(F[9][:, hh0:hh0 + n, :], pv, AF.Sqrt), kk2b)
        P.ts('dve', F[9][:], F[9][:], 1e-12, None, ALU.max)
        P.recip(F[9][:], F[9][:])
        P.tt('dve', kk[:], kk[:], F[9][:], ALU.mult)
        P.tt('dve', kmod[:], aa[:], bl(vec[:, KA, :], TS), ALU.mult)
        P.tt('dve', kmod[:], kmod[:], bl(omka[:], TS), ALU.add)
        P.tt('pool', kmod[:], kmod[:], kp[:], ALU.mult)
        yield
        cl, eW, eWm = F[9], F[10], F[11]
        for hh in range(3):
            P.op('dve', lambda e, hh=hh: e.tensor_tensor_scan(cl[:, hh, :], rm[:], ld[:, hh, :], 0.0, ALU.mult, ALU.add),
                 [rm[:], ld[:, hh, :]], [cl[:, hh, :]])
        P.act(eW[:], cl[:], AF.Exp)
        P.tt('dve', eWm[:], cl[:], ld[:], ALU.subtract)
        P.act(eWm[:], eWm[:], AF.Exp)
        P.copy('act', WL[:], eW[:, :, 63:TS:64])
        yield
        At, Rt, Bt, Kt = B[2], B[3], B[4], B[5]
        P.stt('dve', At[:], eWm[:], -1.0, kk[:], ALU.mult, ALU.mult)
        P.tt('pool', Rt[:], rp[:], eW[:], ALU.mult)
        P.act(cl[:], cl[:], AF.Exp, scale=-1.0)
        P.tt('dve', eWm[:], kk[:], aa[:], ALU.mult)
        P.tt('dve', Bt[:], eWm[:], cl[:], ALU.mult)
        P.tt('pool', Kt[:], kmod[:], cl[:], ALU.mult)
        yield
        BV = F[0]
        rkrb = B[1]
        P.tt('dve', eWm[:], rp[:], kmod[:], ALU.mult)
        P.tt('dve', rkrb[:], eWm[:], bl(vec[:, RK, :], TS), ALU.mult)
        colsum(lambda hh0, n, pv: P.tt('dve', BV[:, hh0:hh0 + n, :], pv, vp[:, hh0:hh0 + n, :], ALU.mult), rkrb)
        vpb = B[0]
        P.copy('act', vpb[:], vp[:])
        yield
        Vtok, Btok, Ktok = B[6], B[7], B[8]
        for (src, dst) in ((vpb, Vtok), (Bt, Btok), (Kt, Ktok)):
            pt = next_psT(K)
            for g in range(2):
                for hh in range(3):
                    for c in range(NC):
                        o = (hh * NC + c) * 64
                        P.tr(pt[hp(g), o:o + 64], src[hp(g), hh, c * 64:(c + 1) * 64], K.identb[hp(g), hp(g)])
            P.copy('act', dst[:], pt[:, 0:3 * TS].rearrange("p (h t) -> p h t", h=3))
        yield
        NTm, Nm, Arb, Aak, Ark = B[9], B[10], B[11], B[12], B[13]
        for (dst, lt, rt, mi) in ((NTm, Bt, At, 0), (Nm, At, Bt, 2), (Arb, Bt, Rt, 1), (Aak, Kt, At, 0), (Ark, Kt, Rt, 1)):
            def ev(hh0, n, pv, dst=dst, mi=mi):
                P.tt('dve', dst[:, hh0:hh0 + n, :], pv, msk[:, mi, 0:n].rearrange("p h c t -> p h (c t)"), ALU.mult)
            mm_blocks(lt, rt, ev)
            yield
        yield
        Tt32, Ttb = F[1], B[14]
        for hh0, n in GR:
            P.tt('dve', Tt32[:, hh0:hh0 + n, :], NTm[:, hh0:hh0 + n, :], itl[:, 0:n].rearrange("p h c t -> p h (c t)"), ALU.add)
        P.copy('act', Ttb[:], Tt32[:])
        cN, cNT, nN, nNT = Nm, NTm, B[15], B[1]
        for lvl in range(5):
            last = (lvl == 4)
            def evN(hh0, n, pv, d=nN):
                P.copy('act', d[:, hh0:hh0 + n, :], pv)
            mm_blocks(cNT, cN, evN)
            if not last:
                def evNT(hh0, n, pv, d=nNT):
                    P.copy('act', d[:, hh0:hh0 + n, :], pv)
                mm_blocks(cN, cNT, evNT)
            def evT(hh0, n, pv):
                P.tt('dve', Tt32[:, hh0:hh0 + n, :], Tt32[:, hh0:hh0 + n, :], pv, ALU.add)
                P.copy('act', Ttb[:, hh0:hh0 + n, :], Tt32[:, hh0:hh0 + n, :])
            mm_blocks(nN, Ttb, evT)
            cN, cNT, nN, nNT = nN, nNT, cN, cNT
            yield
        yield
        Yt = F[2]
        for c in range(NC):
            cc = slice(c * 64, (c + 1) * 64)
            pz = next_ps(K)
            for g in range(2):
                for hh in range(3):
                    o = slice(hh * 64, (hh + 1) * 64)
                    P.mm(pz[hp(g), o], At[hp(g), hh, cc], STb[hp(g), hh, :], start=(hh == 0))
                    P.mm(pz[hp(g), o], Aak[hp(g), hh, cc], Vtok[hp(g), hh, cc], start=False)
            P.copy('act', Zs[:].rearrange("p h v -> p (h v)"), pz[:, 0:192])
            pu_ = next_ps(K)
            for g in range(2):
                for hh in range(3):
                    o = slice(hh * 64, (hh + 1) * 64)
                    P.mm(pu_[hp(g), o], Ttb[hp(g), hh, cc], Zs[hp(g), hh, :], start=(hh == 0))
            P.copy('act', Us[:].rearrange("p h v -> p (h v)"), pu_[:, 0:192])
            py = next_ps(K)
            for g in range(2):
                for hh in range(3):
                    o = slice(hh * 64, (hh + 1) * 64)
                    P.mm(py[hp(g), o], STb[hp(g), hh, :], Rt[hp(g), hh, cc], start=(hh == 0))
                    P.mm(py[hp(g), o], Us[hp(g), hh, :], Arb[hp(g), hh, cc], start=False)
                    P.mm(py[hp(g), o], Vtok[hp(g), hh, cc], Ark[hp(g), hh, cc], start=False)
            P.copy('act', Yt[:, :, cc], py[:, 0:192].rearrange("p (h t) -> p h t", h=3))
            pn = next_ps(K)
            for g in range(2):
                for hh in range(3):
                    o = slice(hh * 64, (hh + 1) * 64)
                    P.mm(pn[hp(g), o], Btok[hp(g), hh, cc], Us[hp(g), hh, :], start=(hh == 0))
                    P.mm(pn[hp(g), o], Ktok[hp(g), hh, cc], Vtok[hp(g), hh, cc], start=False)
            P.tt('dve', ST[:], ST[:], pn[:, 0:192].rearrange("p (h v) -> p h v", h=3), ALU.add)
            P.tt('dve', ST[:], ST[:], bl(WL[:, :, c], 64), ALU.mult)
            P.copy('act', STb[:], ST[:])
            yield
        yield
        yc, sq = F[3], F[5]
        Ytb, sqb = B[4], B[5]
        P.copy('act', Ytb[:], Yt[:])
        colsum(lambda hh0, n, pv: P.stt('dve', yc[:, hh0:hh0 + n, :], pv, -1.0 / 64, Yt[:, hh0:hh0 + n, :], ALU.mult, ALU.add), Ytb)
        P.tt('pool', sqb[:], yc[:], yc[:], ALU.mult)
        colsum(lambda hh0, n, pv: P.act(sq[:, hh0:hh0 + n, :], pv, AF.Sqrt, bias=64e-5, scale=1.0 / 64), sqb)
        P.recip(sq[:], sq[:])
        P.tt('dve', yc[:], yc[:], sq[:], ALU.mult)
        P.tt('dve', yc[:], yc[:], bl(vec[:, LNW, :], TS), ALU.mult)
        P.tt('pool', yc[:], yc[:], bl(vec[:, LNB, :], TS), ALU.add)
        P.tt('pool', yc[:], yc[:], BV[:], ALU.add)
        P.tt('dve', yc[:], yc[:], gg[:], ALU.mult)
        for h in range(6):
            g, hh = divmod(h, 3)
            r0 = 256 + h * 64
            P.dma(('sp', 'pool')[h % 2], K.MIXT[r0:r0 + 64, t0:t0 + TS], yc[hp(g), hh, :], writes=[dkey('MIXT', r0, r0 + 64, t0, t0 + TS)])

    NSC = S // TS
    active = []
    nxt = 0
    done_steps = 0
    TOT = NSC * 31.0
    while nxt < NSC or active:
        if nxt < NSC and len(active) < RW2_NSETS and (not active or active[-1][1] >= RW2_LAG):
            active.append([superchunk(nxt, sets[nxt % RW2_NSETS]), 0])
            nxt += 1
        for a in list(active):
            try:
                next(a[0])
                a[1] += 1
                done_steps += 1
            except StopIteration:
                active.remove(a)
        yield min(0.999, done_steps / TOT)


def phase_rwkv2(K, l):
    with ExitStack() as st:
        for _ in gen_rwkv2(K, l, st):
            pass
        K.P.emit()
```
